# Optimizing a Trainium2 kernel written in Bass

```python
import jax, jax.numpy as jnp
from jax import lax
import numpy as np

D_MODEL = 1024
BATCH = 8
SEQ = 4096
DEPTH = 4

N_MIXERS = 2
N_POOL_LAYERS = (DEPTH + N_MIXERS - 1) // N_MIXERS
N_HGRN_LAYERS = DEPTH // N_MIXERS
POOL_WINDOWS = (2, 4, 8, 16)
POOL_GROUPS = len(POOL_WINDOWS)
POOL_GROUP_WIDTH = D_MODEL // POOL_GROUPS
HGRN_EXPAND = 128
HGRN_HEADS = D_MODEL // HGRN_EXPAND
HGRN_HEAD_DIM = D_MODEL // HGRN_HEADS
HGRN_CHUNK = 32
D_FF = ((8 * D_MODEL // 3 + 255) // 256) * 256
EPS = 1e-6

kernel_name = "hybrid_pool_hgrn2_adaln_trunk"


def _rmsnorm(x, gain):
    xf = x.astype(jnp.float32)
    y = xf * lax.rsqrt(jnp.mean(xf * xf, axis=-1, keepdims=True) + EPS)
    return (y * gain.astype(jnp.float32)).astype(x.dtype)


def _modulate(h, shift, scale):
    return h * (1 + scale[:, None, :]) + shift[:, None, :]


def _pool_mixer(h, w_grp, ch_scale):
    B, T, D = h.shape
    hf = h.astype(jnp.float32)
    cs = jnp.cumsum(hf, axis=1)
    pos = jnp.arange(T)
    outs = []
    for g, w in enumerate(POOL_WINDOWS):
        sl = slice(g * POOL_GROUP_WIDTH, (g + 1) * POOL_GROUP_WIDTH)
        csg = cs[..., sl]
        lag = jnp.pad(csg, ((0, 0), (w, 0), (0, 0)))[:, :T]
        cnt = jnp.minimum(pos + 1, w).astype(jnp.float32)[None, :, None]
        outs.append((csg - lag) / cnt - hf[..., sl])
    d = jnp.stack(outs, axis=2)
    y = jnp.einsum('btgc,gce->btge', d, w_grp.astype(jnp.float32)).reshape(B, T, D)
    return (y * ch_scale.astype(jnp.float32)).astype(h.dtype)


def _gla_chunk_scan(q, k, v, log_f):
    NC, B, H, C, DK = q.shape
    DV = v.shape[-1]
    causal = jnp.tril(jnp.ones((C, C), dtype=bool))[:, :, None]

    def step(S, inp):
        qc, kc, vc, gc = inp
        b = jnp.cumsum(gc, axis=-2)
        o_inter = jnp.einsum('bhik,bhkv->bhiv', qc * jnp.exp(b), S)
        rel = b[:, :, :, None, :] - b[:, :, None, :, :]
        decay = jnp.where(causal, jnp.exp(jnp.where(causal, rel, 0.0)), 0.0)
        att = jnp.einsum('bhik,bhjk,bhijk->bhij', qc, kc, decay)
        o_intra = jnp.einsum('bhij,bhjv->bhiv', att, vc)
        b_last = b[:, :, -1:, :]
        S_new = jnp.exp(b_last[:, :, 0, :])[..., None] * S + jnp.einsum(
            'bhjk,bhjv->bhkv', kc * jnp.exp(b_last - b), vc)
        return S_new, o_inter + o_intra

    S0 = jnp.zeros((B, H, DK, DV), dtype=jnp.float32)
    _, o = lax.scan(step, S0, (q, k, v, log_f))
    return o


def _hgrn2_mixer(h, w_in, lb, norm_gain, w_out):
    B, T, D = h.shape
    NC = T // HGRN_CHUNK
    proj = h @ w_in
    q, z, v, og = jnp.split(proj, 4, axis=-1)
    zf = z.astype(jnp.float32)
    log_f = jax.nn.log_sigmoid(zf) + jnp.log1p(lb * jnp.exp(-zf))
    k = (1 - lb) * jax.nn.sigmoid(-zf)

    def heads(a):
        return a.astype(jnp.float32).reshape(B, NC, HGRN_CHUNK, HGRN_HEADS, -1).transpose(1, 0, 3, 2, 4)

    o = _gla_chunk_scan(heads(q), heads(k), heads(v), heads(log_f))
    o = o.transpose(1, 0, 3, 2, 4).reshape(B, T, HGRN_HEADS, HGRN_HEAD_DIM)
    o = o * lax.rsqrt(jnp.mean(o * o, axis=-1, keepdims=True) + EPS)
    o = o.reshape(B, T, D) * norm_gain.astype(jnp.float32)
    o = o * jax.nn.silu(og.astype(jnp.float32))
    return (o.astype(h.dtype)) @ w_out


def _swiglu(h, w_in, w_out):
    gu = h @ w_in
    a, b = jnp.split(gu, 2, axis=-1)
    return (jax.nn.silu(a) * b) @ w_out


def setup_inputs(seed: int = 0) -> dict:
    key = jax.random.key(seed)
    ks = jax.random.split(key, 16)
    D, F = D_MODEL, D_FF
    nrm = jax.random.normal
    return {
        "x": nrm(ks[0], (BATCH, SEQ, D), jnp.float32),
        "c": nrm(ks[1], (BATCH, D), jnp.float32),
        "norm_mix_gain": 1.0 + 0.02 * nrm(ks[2], (DEPTH, D), jnp.float32),
        "norm_ffn_gain": 1.0 + 0.02 * nrm(ks[3], (DEPTH, D), jnp.float32),
        "ada_w": 0.5 * D ** -0.5 * nrm(ks[4], (DEPTH, D, 6 * D), jnp.float32),
        "ada_b": 0.02 * nrm(ks[5], (DEPTH, 6 * D), jnp.float32),
        "pool_w": POOL_GROUP_WIDTH ** -0.5 * nrm(ks[6], (N_POOL_LAYERS, POOL_GROUPS, POOL_GROUP_WIDTH, POOL_GROUP_WIDTH), jnp.float32),
        "pool_scale": 1.0 + 0.1 * nrm(ks[7], (N_POOL_LAYERS, D), jnp.float32),
        "hgrn_w_in": D ** -0.5 * nrm(ks[8], (N_HGRN_LAYERS, D, 4 * D), jnp.float32),
        "hgrn_lb_logits": nrm(ks[9], (N_HGRN_LAYERS, D), jnp.float32),
        "hgrn_norm_gain": 1.0 + 0.02 * nrm(ks[10], (N_HGRN_LAYERS, D), jnp.float32),
        "hgrn_w_out": D ** -0.5 * nrm(ks[11], (N_HGRN_LAYERS, D, D), jnp.float32),
        "ffn_w_in": D ** -0.5 * nrm(ks[12], (DEPTH, D, 2 * F), jnp.float32),
        "ffn_w_out": F ** -0.5 * nrm(ks[13], (DEPTH, F, D), jnp.float32),
        "final_gain": 1.0 + 0.02 * nrm(ks[14], (D,), jnp.float32),
    }


def reference(x, c, norm_mix_gain, norm_ffn_gain, ada_w, ada_b, pool_w, pool_scale,
              hgrn_w_in, hgrn_lb_logits, hgrn_norm_gain, hgrn_w_out, ffn_w_in, ffn_w_out,
              final_gain):
    sm = jax.nn.softmax(hgrn_lb_logits.astype(jnp.float32), axis=0)
    lower_bounds = jnp.cumsum(sm, axis=0) - sm[0]
    c_act = jax.nn.silu(c)
    for i in range(DEPTH):
        mod = c_act @ ada_w[i] + ada_b[i]
        sh1, sc1, g1, sh2, sc2, g2 = jnp.split(mod, 6, axis=-1)
        h = _modulate(_rmsnorm(x, norm_mix_gain[i]), sh1, sc1)
        j = i // N_MIXERS
        if i % N_MIXERS == 0:
            y = _pool_mixer(h, pool_w[j], pool_scale[j])
        else:
            y = _hgrn2_mixer(h, hgrn_w_in[j], lower_bounds[j], hgrn_norm_gain[j], hgrn_w_out[j])
        x = x + g1[:, None, :] * y
        h = _modulate(_rmsnorm(x, norm_ffn_gain[i]), sh2, sc2)
        x = x + g2[:, None, :] * _swiglu(h, ffn_w_in[i], ffn_w_out[i])
    return _rmsnorm(x, final_gain)
```

```python
import contextlib
import numpy as np
import concourse.bass as bass
import concourse.mybir as mybir
from concourse.bass_utils import run_bass_kernel_spmd

F32 = mybir.dt.float32
BF16 = mybir.dt.bfloat16
AF = mybir.ActivationFunctionType
ALU = mybir.AluOpType

D = 1024
KC = 8
DFF = 2816
NF = 22
TT = 1024
HALO = 16
NBLK = TT // 128
EPS = 1e-6
NB_RING = 3
SLOT = 4096
POOL_W = (2, 4, 8, 16)


class Sched:
    def __init__(self, nc, stack):
        self.nc = nc
        self.stack = stack
        self.engs = {'pe': nc.tensor, 'act': nc.scalar, 'dve': nc.vector,
                     'pool': nc.gpsimd, 'sp': nc.sync}
        self.semh = {}
        self.cnt = {}
        for e in self.engs:
            self.semh[e] = stack.enter_context(nc.semaphore("prog_" + e))
            self.cnt[e] = 0
        self.seen = {e: {} for e in self.engs}
        self.last_w = {}
        self.readers = {}
        self.n_wait = 0
        self.n_ins = 0
        self.eng_free = {e: 0.0 for e in self.engs}
        self.tok_w = {}
        self.tok_r = {}

    class _Rec:
        def __getattr__(self, name):
            return lambda *a, **k: (name, a, k)

    @staticmethod
    def _fsz(ap):
        n = 1
        for d in ap.shape[1:]:
            n *= int(d)
        return n

    def est_dur(self, eng, fn, dma):
        if dma is not None:
            return 6.0
        try:
            name, a, k = fn(Sched._Rec())
            if name == 'matmul':
                return 0.07 + Sched._fsz(k['rhs']) / 1950.0
            if name == 'transpose':
                return 0.13
            out = k.get('out', a[0] if a else None)
            n = Sched._fsz(out)
            if name == 'activation':
                return 0.25 + n / 1400.0
            if name == 'tensor_tensor_scan':
                return 0.12 + 2.0 * n / 960.0
            return 0.1 + n / 960.0
        except Exception:
            return 0.5

    def earliest(self, eng, reads, writes):
        t = self.eng_free[eng]
        for r in reads:
            v = self.tok_w.get(r, 0.0)
            if v > t:
                t = v
        for w in writes:
            v = self.tok_w.get(w, 0.0)
            if v > t:
                t = v
            v = self.tok_r.get(w, 0.0)
            if v > t:
                t = v
        return t

    def op(self, eng, fn, reads=(), writes=(), dma=None):
        dur = self.est_dur(eng, fn, dma)
        st = self.earliest(eng, reads, writes)
        fin = st + dur + 0.12
        self.eng_free[eng] = (st + 0.1) if dma is not None else (st + dur)
        for t in reads:
            if fin > self.tok_r.get(t, 0.0):
                self.tok_r[t] = fin
        for t in writes:
            self.tok_w[t] = fin
            self.tok_r[t] = 0.0
        need = {}
        own_ok = (dma is None)
        for t in reads:
            w = self.last_w.get(t)
            if w is not None:
                k, v = w
                if not (own_ok and k == eng and eng == 'pe'):
                    if v > need.get(k, 0):
                        need[k] = v
            if type(t) is tuple and t[0] == 'ps':
                rd = self.readers.get(t)
                if rd:
                    for k, v in rd.items():
                        if k != eng and v > need.get(k, 0):
                            need[k] = v
        for t in writes:
            w = self.last_w.get(t)
            if w is not None:
                k, v = w
                if not (own_ok and k == eng and eng == 'pe'):
                    if v > need.get(k, 0):
                        need[k] = v
            rd = self.readers.get(t)
            if rd:
                for k, v in rd.items():
                    if own_ok and k == eng and eng == 'pe':
                        continue
                    if v > need.get(k, 0):
                        need[k] = v
        e = self.engs[eng]
        seen = self.seen[eng]
        for k, v in need.items():
            if seen.get(k, 0) >= v:
                continue
            e.wait_ge(self.semh[k], v)
            self.n_wait += 1
            seen[k] = v
        ins = fn(e)
        self.n_ins += 1
        if dma is None:
            self.cnt[eng] += 1
            ins.then_inc(self.semh[eng], 1)
            me = (eng, self.cnt[eng])
        else:
            if dma not in self.semh:
                self.semh[dma] = self.stack.enter_context(self.nc.semaphore("dma_" + dma))
                self.cnt[dma] = 0
            self.cnt[dma] += 16
            ins.then_inc(self.semh[dma], 16)
            me = (dma, self.cnt[dma])
        for t in reads:
            rd = self.readers.setdefault(t, {})
            if me[1] > rd.get(me[0], 0):
                rd[me[0]] = me[1]
        for t in writes:
            self.last_w[t] = me
            self.readers[t] = {}
        return me

    def wait_all(self, eng, toks):
        e = self.engs[eng]
        for t in toks:
            w = self.last_w.get(t)
            if w is None:
                continue
            if self.seen[eng].get(w[0], 0) >= w[1]:
                continue
            e.wait_ge(self.semh[w[0]], w[1])
            self.seen[eng][w[0]] = w[1]


def layer_kinds(depth):
    return ['pool' if i % 2 == 0 else 'hgrn' for i in range(depth)]


def pass_chunks(depth):
    out = []
    for i, kind in enumerate(layer_kinds(depth)):
        if kind == 'pool':
            out.append(('pool', i, 0, 2048))
        else:
            for hd in range(8):
                out.append(('hin', i, hd, 4096))
            for dh in range(2):
                out.append(('hout', i, dh, 4096))
        for pi in range(NF // 2):
            out.append(('fin', i, pi, 4096))
        for dt in range(KC):
            out.append(('fout', i, dt, NF * 128))
    return out


def host_chunk(kind, i, idx, inp):
    j = i // 2
    if kind == 'pool':
        w = inp['pool_w'][j]
        a = w.reshape(4, 2, 128, 256).transpose(2, 0, 1, 3)
        return a.reshape(128, 2048)
    if kind == 'hin':
        w = inp['hgrn_w_in'][j]
        a = w.reshape(8, 128, 4, 8, 128)[:, :, :, idx, :]
        return a.transpose(1, 0, 2, 3).reshape(128, 4096)
    if kind == 'hout':
        w = inp['hgrn_w_out'][j]
        a = w.reshape(8, 128, 2, 4, 128)[:, :, idx, :, :]
        return a.transpose(1, 2, 0, 3).reshape(128, 4096)
    if kind == 'fin':
        w = inp['ffn_w_in'][i]
        wr = w.reshape(8, 128, 2, NF, 128)
        a = wr[:, :, :, 2 * idx:2 * idx + 2, :]
        return a.transpose(1, 3, 0, 2, 4).reshape(128, 4096)
    if kind == 'fout':
        w = inp['ffn_w_out'][i]
        a = w.reshape(NF, 128, 8, 128)[:, :, idx, :]
        return a.transpose(1, 0, 2).reshape(128, NF * 128)
    raise ValueError(kind)


def cols_layout(depth):
    npool = (depth + 1) // 2
    nh = depth // 2
    off = {}
    o = 0
    for name, n in (('c', 8), ('gmix', depth * 8), ('gffn', depth * 8), ('pscale', npool * 8),
                    ('lbl', 2 * 8 * max(nh, 1)), ('hng', max(nh, 1) * 8), ('fgain', 8),
                    ('adab', depth * 48)):
        off[name] = o
        o += n
    return off, o


NCONST = 128 + 128 + 64


def build_nc(T, depth):
    ntile = T // TT
    kinds = layer_kinds(depth)
    coff, ncols = cols_layout(depth)
    chunks = pass_chunks(depth)
    pass_cols = sum(c[3] for c in chunks)
    a_cols = depth * 12 * 4096

    nc = bass.Bass("TRN2", target_bir_lowering=False)
    xT_d = nc.dram_tensor("xT", [D, T], F32, kind="ExternalInput").ap()
    ws_d = nc.dram_tensor("wstream", [128, pass_cols], F32, kind="ExternalInput").ap()
    as_d = nc.dram_tensor("astream", [128, a_cols], F32, kind="ExternalInput").ap()
    cols_d = nc.dram_tensor("cols", [128, ncols], F32, kind="ExternalInput").ap()
    const_d = nc.dram_tensor("consts", [128, NCONST], F32, kind="ExternalInput").ap()
    out_d = nc.dram_tensor("outT", [D, T], F32, kind="ExternalOutput").ap()

    with contextlib.ExitStack() as st:
        S = Sched(nc, st)

        def sb(name, shape, dt):
            return st.enter_context(nc.sbuf_tensor(name, shape, dt))

        xT = sb("xT_sb", [128, KC, TT], F32)
        hT = sb("hT_sb", [128, KC, HALO + TT], BF16)
        sq = sb("sq_sb", [128, KC, TT], BF16)
        rstd = sb("rstd_sb", [128, TT], F32)
        ntmp = [sb("ntmp%d" % i, [128, TT], F32) for i in range(2)]
        ring = [sb("ring%d" % i, [128, SLOT], BF16) for i in range(NB_RING)]
        ARENA_B = 65536
        arena = sb("arena", [128, ARENA_B // 2], BF16)
        sg = [sb("sg%d" % i, [128, TT], BF16) for i in range(2)]
        cols = sb("cols_sb", [128, ncols], F32)
        consts = sb("consts_sb", [128, NCONST], F32)
        ident = sb("ident_bf", [128, 128], BF16)
        ones_bf = sb("ones_bf", [128, 128], BF16)
        mask32 = sb("mask32", [128, 512], BF16)
        mask128 = sb("mask128", [128, 512], BF16)
        epscol = sb("epscol", [128, 1], F32)
        one11 = sb("one11", [1, 1], F32)
        cact = sb("cact", [128, KC], BF16)
        rowbuf = [sb("rowbuf%d" % i, [1, 512], F32) for i in range(2)]
        modc = sb("modc", [128, depth, 48], F32)
        dcol = sb("dcol", [128, depth, 6, KC], F32)
        nh = max(depth // 2, 1)
        npool = (depth + 1) // 2
        lbc = sb("lbc", [128, nh, 2, 8], F32)
        Sst = sb("S_state", [128, nh, 8, 128], F32)
        Sbf8 = sb("S_bf8", [128, NBLK, 128], BF16)
        S2 = sb("S2", [128, 128], F32)
        halo = sb("halo", [128, npool, KC, HALO], BF16)
        OG = sb("og_sb", [128, KC, TT], BF16)
        tmp16 = sb("tmp16", [128, 2, HALO], F32)

        ps = [st.enter_context(nc.psum_tensor("psb%d" % i, [128, 512], F32)) for i in range(8)]

        def aview(byte_off, shape, dt):
            esz = 4 if dt == F32 else 2
            n = int(np.prod(shape[1:]))
            a = arena[:, byte_off // 2: byte_off // 2 + n * esz // 2]
            if dt == F32:
                a = a.bitcast(F32)
            if len(shape) == 3:
                a = a.rearrange("p (a b) -> p a b", b=shape[2])
            elif len(shape) == 4:
                a = a.rearrange("p (a b c) -> p a b c", b=shape[2], c=shape[3])
            return a

        act_v = aview(0, [128, NF, TT], BF16)
        sA = aview(0, [128, 2, HALO + TT], F32)
        sB = aview(8320, [128, 2, HALO + TT], F32)
        def rawview(base2d, byte_off, shape, dt):
            esz = 4 if dt == F32 else 2
            n = int(np.prod(shape[1:]))
            a = base2d[:, byte_off // 2: byte_off // 2 + n * esz // 2]
            if dt == F32:
                a = a.bitcast(F32)
            if len(shape) == 3:
                a = a.rearrange("p (a b) -> p a b", b=shape[2])
            return a

        hg = {}
        o = 0
        for nm in ('e0', 'e1', 'l1', 'l2', 'kk'):
            hg[nm] = aview(o, [128, TT], F32)
            o += 4096
        hg['bl'] = ntmp[0]
        hg['bb'] = ntmp[1]
        hgp = []
        for par in range(2):
            d = {}
            for nm in ('ql', 'qb', 'khat', 'qsb'):
                d[nm] = aview(o, [128, TT], BF16)
                o += 2048
            d['KH'] = aview(o, [128, NBLK, 512], BF16); o += 8192
            d['e'] = hg['e%d' % par]
            hgp.append(d)
        hg3 = []
        for m3 in range(3):
            d = {}
            d['sog'] = aview(o, [128, TT], BF16); o += 2048
            d['VT'] = aview(o, [128, NBLK, 128], BF16); o += 2048
            hg3.append(d)
        assert o <= ARENA_B, o
        sqflat = sq[:, :, :].rearrange("p a b -> p (a b)")
        KT = rawview(sqflat, 0, [128, NBLK, 128], BF16)
        AT = rawview(sqflat, 2048, [128, NBLK, 128], BF16)
        osq_b = rawview(sqflat, 4096, [128, TT], BF16)
        rs_b = rawview(sqflat, 6144, [128, TT], F32)
        on_b = rawview(sqflat, 10240, [128, TT], F32)
        dcy = sb("dcy", [128, 2, NBLK], F32)
        Gs = sb("Gs", [128, NBLK, 6], F32)

        ident_f = consts[:, 0:128]
        cmask = consts[:, 128:256]
        rcnt = consts[:, 256:320].rearrange("p (g t) -> p g t", t=HALO)

        stream = []
        for i in range(depth):
            for nb in range(12):
                o0 = (i * 12 + nb) * 4096
                stream.append((as_d[:, o0:o0 + 4096], 4096))
        for t in range(ntile):
            o0 = 0
            for (_k, _i, _x, n) in chunks:
                stream.append((ws_d[:, o0:o0 + n], n))
                o0 += n
        wstate = {'issued': 0, 'next': 0}

        def w_issue():
            k = wstate['issued']
            if k >= len(stream):
                return
            src, n = stream[k]
            s = k % NB_RING
            S.op('pool', lambda e: e.dma_start(out=ring[s][:, 0:n], in_=src),
                 writes=[('w', s)], dma='ring%d' % s)
            wstate['issued'] += 1

        def w_next():
            k = wstate['next']
            while wstate['issued'] <= k:
                w_issue()
            wstate['next'] += 1
            s = k % NB_RING
            return ring[s], ('w', s)

        def w_release():
            w_issue()

        psrot = {'i': 0}

        def next_bank():
            b = psrot['i'] % 8
            psrot['i'] += 1
            return b

        S.op('sp', lambda e: e.dma_start(out=cols[:], in_=cols_d), writes=['cols'], dma='c0')
        S.op('sp', lambda e: e.dma_start(out=consts[:], in_=const_d), writes=['consts'], dma='c1')
        for _ in range(NB_RING):
            w_issue()
        S.op('dve', lambda e: e.memset(ones_bf[:], 1.0), writes=['ones'])
        S.op('dve', lambda e: e.memset(epscol[:], EPS), writes=['eps'])
        S.op('dve', lambda e: e.memset(one11[:], 1.0), writes=['one11'])
        S.op('dve', lambda e: e.memset(mask32[:], 1.0), writes=['m32'])
        S.op('dve', lambda e: e.memset(mask32[:].rearrange("p (c j) -> p c j", j=32)[:, :, 0:1], 0.0),
             writes=['m32'])
        S.op('dve', lambda e: e.memset(mask128[:], 1.0), writes=['m128'])
        S.op('dve', lambda e: e.memset(mask128[:].rearrange("p (c j) -> p c j", j=128)[:, :, 0:1], 0.0),
             writes=['m128'])
        S.op('dve', lambda e: e.memset(halo[:], 0.0), writes=[('halo', jj) for jj in range(npool)])
        S.op('dve', lambda e: e.memset(Sst[:], 0.0), writes=[('S', hh) for hh in range(8)])
        S.op('dve', lambda e: e.tensor_copy(out=ident[:], in_=ident_f), reads=['consts'], writes=['ident'])
        S.op('act', lambda e: e.activation(out=cact[:], in_=cols[:, coff['c']:coff['c'] + 8], func=AF.Silu),
             reads=['cols'], writes=['cact'])

        for i in range(depth):
            bcols = 7 if i % 2 == 0 else 6
            for nb in range(12):
                slot, wt = w_next()
                wv = slot[:, 0:4096].rearrange("p (k n) -> p k n", n=512)
                b = nb % 4
                rb = rowbuf[nb % 2]
                rbt = ('rowbuf', nb % 2)
                for kc in range(KC):
                    S.op('pe', lambda e, kc=kc, b=b, wv=wv: e.matmul(ps[b][0:1, :], lhsT=cact[:, kc:kc + 1], rhs=wv[:, kc, :],
                                                         start=(kc == 0), stop=(kc == KC - 1)),
                         reads=[wt, 'cact'], writes=[('ps', b)])
                S.op('dve', lambda e, b=b, rb=rb: e.tensor_copy(out=rb[0:1, :], in_=ps[b][0:1, :]),
                     reads=[('ps', b)], writes=[rbt])
                for jj in range(4):
                    jx = nb * 4 + jj
                    S.op('pe', lambda e, jj=jj, jx=jx, rb=rb, bcols=bcols: e.matmul(
                        ps[bcols][:, jx:jx + 1], lhsT=rb[0:1, jj * 128:(jj + 1) * 128],
                        rhs=one11[0:1, 0:1], start=True, stop=True),
                        reads=[rbt, 'one11'], writes=[('ps', bcols)])
                w_release()
            ab = cols[:, coff['adab'] + i * 48: coff['adab'] + i * 48 + 48]
            S.op('dve', lambda e, i=i, ab=ab, bcols=bcols: e.tensor_tensor(out=modc[:, i, :], in0=ps[bcols][:, 0:48], in1=ab,
                                                                 op=ALU.add),
                 reads=[('ps', bcols), 'cols'], writes=['modc'])
            gm = cols[:, coff['gmix'] + i * 8: coff['gmix'] + i * 8 + 8]
            gf = cols[:, coff['gffn'] + i * 8: coff['gffn'] + i * 8 + 8]
            S.op('dve', lambda e: e.scalar_tensor_tensor(out=dcol[:, i, 0, :], in0=modc[:, i, 8:16], scalar=1.0,
                                                         in1=gm, op0=ALU.add, op1=ALU.mult),
                 reads=['modc', 'cols'], writes=['dcol'])
            S.op('dve', lambda e: e.tensor_copy(out=dcol[:, i, 1, :], in_=modc[:, i, 0:8]),
                 reads=['modc'], writes=['dcol'])
            if kinds[i] == 'pool':
                psc = cols[:, coff['pscale'] + (i // 2) * 8: coff['pscale'] + (i // 2) * 8 + 8]
                S.op('dve', lambda e: e.tensor_tensor(out=dcol[:, i, 2, :], in0=modc[:, i, 16:24], in1=psc,
                                                      op=ALU.mult), reads=['modc', 'cols'], writes=['dcol'])
            else:
                S.op('dve', lambda e: e.tensor_copy(out=dcol[:, i, 2, :], in_=modc[:, i, 16:24]),
                     reads=['modc'], writes=['dcol'])
            S.op('dve', lambda e: e.scalar_tensor_tensor(out=dcol[:, i, 3, :], in0=modc[:, i, 32:40], scalar=1.0,
                                                         in1=gf, op0=ALU.add, op1=ALU.mult),
                 reads=['modc', 'cols'], writes=['dcol'])
            S.op('dve', lambda e: e.tensor_copy(out=dcol[:, i, 4, :], in_=modc[:, i, 24:32]),
                 reads=['modc'], writes=['dcol'])
            S.op('dve', lambda e: e.tensor_copy(out=dcol[:, i, 5, :], in_=modc[:, i, 40:48]),
                 reads=['modc'], writes=['dcol'])
        for j in range(depth // 2):
            if j == 0:
                S.op('dve', lambda e: e.memset(lbc[:, 0, 0, :], 0.0), writes=['lbc'])
                S.op('dve', lambda e: e.memset(lbc[:, 0, 1, :], 1.0), writes=['lbc'])
            else:
                l0 = cols[:, coff['lbl']: coff['lbl'] + 8]
                l1 = cols[:, coff['lbl'] + 8: coff['lbl'] + 16]
                S.op('dve', lambda e: e.tensor_tensor(out=lbc[:, j, 0, :], in0=l0, in1=l1, op=ALU.subtract),
                     reads=['cols'], writes=['lbc'])
                S.op('act', lambda e: e.activation(out=lbc[:, j, 0, :], in_=lbc[:, j, 0, :], func=AF.Exp),
                     reads=['lbc'], writes=['lbc'])
                S.op('dve', lambda e: e.tensor_scalar_add(out=lbc[:, j, 0, :], in0=lbc[:, j, 0, :], scalar1=1.0),
                     reads=['lbc'], writes=['lbc'])
                S.op('dve', lambda e: e.reciprocal(out=lbc[:, j, 0, :], in_=lbc[:, j, 0, :]),
                     reads=['lbc'], writes=['lbc'])
                S.op('dve', lambda e: e.tensor_scalar(out=lbc[:, j, 1, :], in0=lbc[:, j, 0, :], scalar1=-1.0,
                                                      scalar2=1.0, op0=ALU.mult, op1=ALU.add),
                     reads=['lbc'], writes=['lbc'])

        XTOK = [('x', kc) for kc in range(KC)]
        HTOK = [('h', kc) for kc in range(KC)]

        def norm_phase(acol, bcol, final=False):
            for kc in range(KC):
                S.op('act', lambda e, kc=kc: e.activation(out=sq[:, kc, :], in_=xT[:, kc, :], func=AF.Square),
                     reads=[('x', kc)], writes=[('sq', kc)])
            bk = [next_bank(), next_bank()]
            for half in range(2):
                for kc in range(KC):
                    S.op('pe', lambda e, kc=kc, half=half: e.matmul(
                        ps[bk[half]][:, :], lhsT=ones_bf[:, :], rhs=sq[:, kc, half * 512:(half + 1) * 512],
                        start=(kc == 0), stop=(kc == KC - 1)),
                        reads=[('sq', kc), 'ones'], writes=[('ps', bk[half])])
            for half in range(2):
                hs = slice(half * 512, (half + 1) * 512)
                S.op('act', lambda e, half=half, hs=hs: e.activation(out=rstd[:, hs], in_=ps[bk[half]][:, :],
                                                                     func=AF.Ln, scale=1.0 / D, bias=epscol[:, 0:1]),
                     reads=[('ps', bk[half]), 'eps'], writes=[('rstd', half)])
                S.op('act', lambda e, hs=hs: e.activation(out=rstd[:, hs], in_=rstd[:, hs], func=AF.Exp, scale=-0.5),
                     reads=[('rstd', half)], writes=[('rstd', half)])
            for kc in range(KC):
                if final:
                    S.op('dve', lambda e, kc=kc: e.scalar_tensor_tensor(
                        out=xT[:, kc, :], in0=xT[:, kc, :], scalar=acol[:, kc:kc + 1], in1=rstd[:, :],
                        op0=ALU.mult, op1=ALU.mult),
                        reads=[('x', kc), ('rstd', 0), ('rstd', 1), 'dcol', 'cols'], writes=[('x', kc)])
                else:
                    nt = ntmp[kc % 2]
                    S.op('dve', lambda e, kc=kc, nt=nt: e.scalar_tensor_tensor(
                        out=nt[:, :], in0=xT[:, kc, :], scalar=acol[:, kc:kc + 1], in1=rstd[:, :],
                        op0=ALU.mult, op1=ALU.mult),
                        reads=[('x', kc), ('rstd', 0), ('rstd', 1), 'dcol'], writes=[('ntmp', kc % 2)])
                    S.op('act', lambda e, kc=kc, nt=nt: e.activation(
                        out=hT[:, kc, HALO:HALO + TT], in_=nt[:, :], func=AF.Identity, bias=bcol[:, kc:kc + 1],
                        scale=1.0),
                        reads=[('ntmp', kc % 2), 'dcol'], writes=[('h', kc)])

        def ffn_phase(i):
            gcol = dcol[:, i, 5, :]
            for pi in range(NF // 2):
                slot, wt = w_next()
                wv = slot[:, 0:4096].rearrange("p (u k w m) -> p u k w m", u=2, k=KC, w=2)
                for u in range(2):
                    ft = 2 * pi + u
                    bg = [next_bank(), next_bank()]
                    bu = [next_bank(), next_bank()]
                    for which, bks in ((0, bg), (1, bu)):
                        for kc in range(KC):
                            for half in range(2):
                                S.op('pe', lambda e, kc=kc, half=half, which=which, bks=bks, u=u: e.matmul(
                                    ps[bks[half]][:, :], lhsT=wv[:, u, kc, which, :],
                                    rhs=hT[:, kc, HALO + half * 512:HALO + (half + 1) * 512],
                                    start=(kc == 0), stop=(kc == KC - 1)),
                                    reads=[wt, ('h', kc)], writes=[('ps', bks[half])])
                    sgb = sg[ft % 2]
                    for half in range(2):
                        hs = slice(half * 512, (half + 1) * 512)
                        S.op('act', lambda e, half=half, hs=hs, sgb=sgb, bg=bg: e.activation(
                            out=sgb[:, hs], in_=ps[bg[half]][:, :], func=AF.Silu),
                            reads=[('ps', bg[half])], writes=[('sg', ft % 2, half)])
                        S.op('dve', lambda e, half=half, hs=hs, sgb=sgb, bu=bu, ft=ft: e.tensor_tensor(
                            out=act_v[:, ft, hs], in0=ps[bu[half]][:, :], in1=sgb[:, hs], op=ALU.mult),
                            reads=[('ps', bu[half]), ('sg', ft % 2, half)], writes=[('act', ft)])
                w_release()
            for dt in range(KC):
                slot, wt = w_next()
                wv = slot[:, 0:NF * 128].rearrange("p (f m) -> p f m", m=128)
                bk = [next_bank(), next_bank()]
                for fk in range(NF):
                    for half in range(2):
                        S.op('pe', lambda e, fk=fk, half=half: e.matmul(
                            ps[bk[half]][:, :], lhsT=wv[:, fk, :], rhs=act_v[:, fk, half * 512:(half + 1) * 512],
                            start=(fk == 0), stop=(fk == NF - 1)),
                            reads=[wt, ('act', fk)], writes=[('ps', bk[half])])
                for half in range(2):
                    hs = slice(half * 512, (half + 1) * 512)
                    S.op('dve', lambda e, half=half, hs=hs, dt=dt: e.scalar_tensor_tensor(
                        out=xT[:, dt, hs], in0=ps[bk[half]][:, :], scalar=gcol[:, dt:dt + 1], in1=xT[:, dt, hs],
                        op0=ALU.mult, op1=ALU.add),
                        reads=[('ps', bk[half]), ('x', dt), 'dcol'], writes=[('x', dt)])
                w_release()

        def pool_phase(i, tile):
            j = i // 2
            gcol = dcol[:, i, 2, :]
            S.op('dve', lambda e: e.tensor_copy(out=hT[:, :, 0:HALO], in_=halo[:, j, :, :]),
                 reads=[('halo', j)], writes=['hh'])
            dd = sq
            for g in range(4):
                w = POOL_W[g]
                c0 = 2 * g
                hv = hT[:, c0:c0 + 2, :]
                rtok = [('h', c0), ('h', c0 + 1), 'hh']
                L = HALO + TT
                S.op('dve', lambda e, hv=hv: e.tensor_tensor(out=sA[:, :, 1:L], in0=hv[:, :, 1:L], in1=hv[:, :, 0:L - 1],
                                                             op=ALU.add), reads=rtok, writes=['sA'])
                cur, curt = sA, 'sA'
                oth, otht = sB, 'sB'
                sh = 1
                lo = 1
                for lev in range(1, g + 1):
                    sh = 2 ** lev
                    lo2 = lo + sh
                    S.op('dve', lambda e, cur=cur, oth=oth, lo2=lo2, sh=sh: e.tensor_tensor(
                        out=oth[:, :, lo2:L], in0=cur[:, :, lo2:L], in1=cur[:, :, lo2 - sh:L - sh], op=ALU.add),
                        reads=[curt], writes=[otht])
                    cur, curt, oth, otht = oth, otht, cur, curt
                    lo = lo2
                for cc in range(2):
                    S.op('dve', lambda e, cur=cur, cc=cc, w=w, c0=c0: e.scalar_tensor_tensor(
                        out=dd[:, c0 + cc, :], in0=cur[:, cc, HALO:L], scalar=1.0 / w, in1=hT[:, c0 + cc, HALO:L],
                        op0=ALU.mult, op1=ALU.subtract),
                        reads=[curt, ('h', c0 + cc)], writes=[('sq', c0 + cc)])
                if tile == 0:
                    for cc in range(2):
                        S.op('dve', lambda e, cur=cur, cc=cc, g=g: e.tensor_tensor(
                            out=tmp16[:, cc, :], in0=cur[:, cc, HALO:2 * HALO], in1=rcnt[:, g, :], op=ALU.mult),
                            reads=[curt, 'consts'], writes=[('t16', cc)])
                        S.op('dve', lambda e, cc=cc, c0=c0: e.tensor_tensor(
                            out=dd[:, c0 + cc, 0:HALO], in0=tmp16[:, cc, :], in1=hT[:, c0 + cc, HALO:2 * HALO],
                            op=ALU.subtract),
                            reads=[('t16', cc), ('h', c0 + cc)], writes=[('sq', c0 + cc)])
            S.op('dve', lambda e: e.tensor_copy(out=halo[:, j, :, :], in_=hT[:, :, TT:TT + HALO]),
                 reads=HTOK + ['hh'], writes=[('halo', j)])
            slot, wt = w_next()
            wv = slot[:, 0:2048].rearrange("p (g c e) -> p g c e", g=4, c=2)
            for g in range(4):
                for ec in range(2):
                    bk = [next_bank(), next_bank()]
                    dt = 2 * g + ec
                    for cc in range(2):
                        for half in range(2):
                            S.op('pe', lambda e, g=g, ec=ec, cc=cc, half=half, bk=bk: e.matmul(
                                ps[bk[half]][:, :], lhsT=wv[:, g, cc, ec * 128:(ec + 1) * 128],
                                rhs=dd[:, 2 * g + cc, half * 512:(half + 1) * 512],
                                start=(cc == 0), stop=(cc == 1)),
                                reads=[wt, ('sq', 2 * g + cc)], writes=[('ps', bk[half])])
                    for half in range(2):
                        hs = slice(half * 512, (half + 1) * 512)
                        S.op('dve', lambda e, half=half, hs=hs, dt=dt, bk=bk: e.scalar_tensor_tensor(
                            out=xT[:, dt, hs], in0=ps[bk[half]][:, :], scalar=gcol[:, dt:dt + 1], in1=xT[:, dt, hs],
                            op0=ALU.mult, op1=ALU.add),
                            reads=[('ps', bk[half]), ('x', dt), 'dcol'], writes=[('x', dt)])
            w_release()

        H2 = [slice(0, 512), slice(512, 1024)]

        def hgrn_phase(i, tile):
            j = i // 2
            gcol = dcol[:, i, 2, :]
            ng = cols[:, coff['hng'] + j * 8: coff['hng'] + j * 8 + 8]
            l1, l2, bl, bb, kk = (hg[n] for n in ('l1', 'l2', 'bl', 'bb', 'kk'))
            ex = l2
            S.op('dve', lambda e: e.memset(AT[:], 0.0), writes=['AT'])
            cur = []

            def OP(eng, fn, reads=(), writes=()):
                cur.append((eng, fn, list(reads), list(writes)))

            def stageP(hd):
                par = hd % 2
                m3 = hd % 3
                e_ = hgp[par]['e']
                qsb = hgp[par]['qsb']
                sog, VT = hg3[m3]['sog'], hg3[m3]['VT']
                slot, wt = w_next()
                wv = slot[:, 0:4096].rearrange("p (k s m) -> p k s m", k=KC, s=4)

                def proj(sidx, banks):
                    for kc in range(KC):
                        for half in range(2):
                            OP('pe', lambda e, kc=kc, half=half: e.matmul(
                                ps[banks[half]][:, :], lhsT=wv[:, kc, sidx, :],
                                rhs=hT[:, kc, HALO + half * 512:HALO + (half + 1) * 512],
                                start=(kc == 0), stop=(kc == KC - 1)),
                                reads=[wt, ('h', kc)], writes=[('ps', banks[half])])
                proj(1, [0, 1])
                for half in range(2):
                    hs = H2[half]
                    OP('act', lambda e, half=half, hs=hs: e.activation(out=e_[:, hs], in_=ps[half][:, :], func=AF.Exp,
                                                                       scale=-1.0),
                       reads=[('ps', half)], writes=[('e', par, half)])
                proj(3, [2, 3])
                for half in range(2):
                    hs = H2[half]
                    OP('act', lambda e, half=half, hs=hs: e.activation(out=sog[:, hs], in_=ps[2 + half][:, :],
                                                                       func=AF.Silu),
                       reads=[('ps', 2 + half)], writes=[('sog', m3)])
                proj(0, [0, 1])
                for half in range(2):
                    hs = H2[half]
                    OP('act', lambda e, half=half, hs=hs: e.activation(out=qsb[:, hs], in_=ps[half][:, :], func=AF.Copy),
                       reads=[('ps', half)], writes=[('qsb', par, half)])
                for blk in range(NBLK):
                    b = 2 + blk // 4
                    for kc in range(KC):
                        OP('pe', lambda e, blk=blk, kc=kc, b=b: e.matmul(
                            ps[b][:, (blk % 4) * 128:(blk % 4) * 128 + 128],
                            lhsT=hT[:, kc, HALO + blk * 128:HALO + blk * 128 + 128], rhs=wv[:, kc, 2, :],
                            start=(kc == 0), stop=(kc == KC - 1)),
                            reads=[wt, ('h', kc)], writes=[('ps', b)])
                for hb in range(2):
                    OP('act', lambda e, hb=hb: e.activation(
                        out=VT[:, 4 * hb:4 * hb + 4, :], in_=ps[2 + hb][:, :].rearrange("p (b m) -> p b m", m=128),
                        func=AF.Copy),
                        reads=[('ps', 2 + hb)], writes=[('VT', m3)])
                cur.append(None)

            def stageE(hd):
                par = hd % 2
                P = hgp[par]
                ql, qb, khat, KH, qsb, e_ = (P[n] for n in ('ql', 'qb', 'khat', 'KH', 'qsb', 'e'))
                lbcol = lbc[:, j, 0, hd:hd + 1]
                omlcol = lbc[:, j, 1, hd:hd + 1]

                def et(half):
                    return ('e', par, half)

                def khtok(half, c_, a_):
                    return ('KH', par, half, c_, a_) if c_ < 4 else ('khat', par, half, a_)
                for half in range(2):
                    hs = H2[half]
                    OP('act', lambda e, hs=hs: e.activation(out=l1[:, hs], in_=e_[:, hs], func=AF.Ln, scale=lbcol,
                                                            bias=1.0),
                       reads=[et(half), 'lbc'], writes=[('l1', half)])
                    OP('act', lambda e, hs=hs: e.activation(out=l2[:, hs], in_=e_[:, hs], func=AF.Ln, bias=1.0,
                                                            scale=1.0),
                       reads=[et(half)], writes=[('l2', half)])
                    OP('dve', lambda e, hs=hs: e.tensor_tensor(out=l1[:, hs], in0=l1[:, hs], in1=l2[:, hs],
                                                               op=ALU.subtract),
                       reads=[('l1', half), ('l2', half)], writes=[('l1', half)])
                    OP('act', lambda e, hs=hs: e.activation(out=l2[:, hs], in_=l2[:, hs], func=AF.Exp, scale=-1.0),
                       reads=[('l2', half)], writes=[('l2', half)])
                    OP('dve', lambda e, hs=hs: e.scalar_tensor_tensor(out=kk[:, hs], in0=e_[:, hs], scalar=omlcol,
                                                                      in1=l2[:, hs], op0=ALU.mult, op1=ALU.mult),
                       reads=[et(half), ('l2', half), 'lbc'], writes=[('kk', half)])
                for half in range(2):
                    hs = H2[half]
                    OP('dve', lambda e, hs=hs: e.tensor_tensor_scan(out=bl[:, hs], data0=mask32[:, :], data1=l1[:, hs],
                                                                   initial=0.0, op0=ALU.mult, op1=ALU.add),
                       reads=[('l1', half), 'm32'], writes=[('bl', half)])
                for half in range(2):
                    hs = H2[half]
                    OP('dve', lambda e, hs=hs: e.tensor_tensor_scan(out=bb[:, hs], data0=mask128[:, :], data1=l1[:, hs],
                                                                   initial=0.0, op0=ALU.mult, op1=ALU.add),
                       reads=[('l1', half), 'm128'], writes=[('bb', half)])
                for half in range(2):
                    hs = H2[half]
                    bl3 = bl[:, hs].rearrange("p (c x) -> p c x", x=32)
                    e3 = e_[:, hs].rearrange("p (c x) -> p c x", x=32)
                    OP('dve', lambda e, bl3=bl3, e3=e3: e.tensor_tensor(
                        out=e3, in0=bl3[:, :, 31:32].to_broadcast([128, 16, 32]), in1=bl3, op=ALU.subtract),
                       reads=[('bl', half), et(half)], writes=[et(half)])
                for half in range(2):
                    hs = H2[half]
                    OP('act', lambda e, hs=hs: e.activation(out=e_[:, hs], in_=e_[:, hs], func=AF.Exp),
                       reads=[et(half)], writes=[et(half)])
                for half in range(2):
                    hs = H2[half]
                    OP('dve', lambda e, hs=hs: e.tensor_tensor(out=l1[:, hs], in0=kk[:, hs], in1=e_[:, hs], op=ALU.mult),
                       reads=[et(half), ('kk', half), ('l1', half)], writes=[('l1', half)])
                for half in range(2):
                    hs = H2[half]
                    bsl = slice(4 * half, 4 * half + 4)
                    Lv = bl[:, hs].rearrange("p (b a x) -> p b a x", a=4, x=32)[:, :, :, 31]
                    gt = ('Gs', half)
                    OP('dve', lambda e, Lv=Lv, bsl=bsl: e.tensor_copy(out=Gs[:, bsl, 0:3], in_=Lv[:, :, 1:4]),
                       reads=[('bl', half)], writes=[gt])
                    OP('dve', lambda e, Lv=Lv, bsl=bsl: e.tensor_tensor(out=Gs[:, bsl, 3:5], in0=Lv[:, :, 1:3],
                                                                        in1=Lv[:, :, 2:4], op=ALU.add),
                       reads=[('bl', half), gt], writes=[gt])
                    OP('dve', lambda e, Lv=Lv, bsl=bsl: e.tensor_tensor(out=Gs[:, bsl, 5:6], in0=Gs[:, bsl, 3:4],
                                                                        in1=Lv[:, :, 3:4], op=ALU.add),
                       reads=[('bl', half), gt], writes=[gt])
                    OP('act', lambda e, bsl=bsl: e.activation(out=Gs[:, bsl, :], in_=Gs[:, bsl, :], func=AF.Exp),
                       reads=[gt], writes=[gt])
                khv = khat[:, :].rearrange("p (b a x) -> p b a x", a=4, x=32)

                def kdst(a_, c_, bsl):
                    if c_ < 4:
                        return KH[:, bsl, 128 * c_ + 32 * a_:128 * c_ + 32 * a_ + 32]
                    return khv[:, bsl, a_, :]
                for (a_, c_) in ((0, 1), (1, 2), (2, 3), (3, 4)):
                    for half in range(2):
                        bsl = slice(4 * half, 4 * half + 4)
                        K1v = l1[:, H2[half]].rearrange("p (b a x) -> p b a x", a=4, x=32)
                        dst = kdst(a_, c_, bsl)
                        OP('act', lambda e, dst=dst, a_=a_, K1v=K1v: e.activation(out=dst, in_=K1v[:, :, a_, :], func=AF.Copy),
                           reads=[('l1', half)], writes=[khtok(half, c_, a_)])
                for (a_, c_, g_) in ((0, 2, 0), (1, 3, 1), (2, 4, 2), (0, 3, 3), (1, 4, 4), (0, 4, 5)):
                    for half in range(2):
                        bsl = slice(4 * half, 4 * half + 4)
                        K1v = l1[:, H2[half]].rearrange("p (b a x) -> p b a x", a=4, x=32)
                        dst = kdst(a_, c_, bsl)
                        OP('dve', lambda e, dst=dst, a_=a_, g_=g_, K1v=K1v, bsl=bsl: e.tensor_tensor(
                            out=dst, in0=K1v[:, :, a_, :], in1=Gs[:, bsl, g_:g_ + 1].to_broadcast([128, 4, 32]), op=ALU.mult),
                           reads=[('l1', half), ('Gs', half)], writes=[khtok(half, c_, a_)])
                for half in range(2):
                    hs = H2[half]
                    OP('act', lambda e, hs=hs: e.activation(out=ex[:, hs], in_=bl[:, hs], func=AF.Exp),
                       reads=[('bl', half)], writes=[('l2', half)])
                for half in range(2):
                    hs = H2[half]
                    OP('dve', lambda e, half=half, hs=hs: e.tensor_tensor(out=ql[:, hs], in0=qsb[:, hs],
                                                                          in1=ex[:, hs], op=ALU.mult),
                       reads=[('qsb', par, half), ('l2', half)], writes=[('ql', par, half)])
                for half in range(2):
                    hs = H2[half]
                    OP('act', lambda e, hs=hs: e.activation(out=e_[:, hs], in_=bl[:, hs], func=AF.Exp, scale=-1.0),
                       reads=[('bl', half), et(half)], writes=[et(half)])
                for c in range(4):
                    for half in range(2):
                        hs = H2[half]
                        bsl = slice(4 * half, 4 * half + 4)
                        OP('dve', lambda e, c=c, hs=hs, bsl=bsl: e.tensor_tensor(
                            out=KH[:, bsl, 160 * c:160 * c + 32],
                            in0=kk[:, hs].rearrange("p (b x) -> p b x", x=128)[:, :, 32 * c:32 * c + 32],
                            in1=e_[:, hs].rearrange("p (b x) -> p b x", x=128)[:, :, 32 * c:32 * c + 32], op=ALU.mult),
                           reads=[('kk', half), et(half)], writes=[khtok(half, c, c)])
                for half in range(2):
                    hs = H2[half]
                    OP('act', lambda e, hs=hs: e.activation(out=ex[:, hs], in_=bb[:, hs], func=AF.Exp),
                       reads=[('bb', half)], writes=[('l2', half)])
                for half in range(2):
                    hs = H2[half]
                    OP('dve', lambda e, half=half, hs=hs: e.tensor_tensor(out=qb[:, hs], in0=qsb[:, hs],
                                                                          in1=ex[:, hs], op=ALU.mult),
                       reads=[('qsb', par, half), ('l2', half)], writes=[('qb', par, half)])
                for half in range(2):
                    hs = H2[half]
                    bsl = slice(4 * half, 4 * half + 4)
                    OP('dve', lambda e, hs=hs, bsl=bsl: e.tensor_copy(
                        out=dcy[:, par, bsl], in_=ex[:, hs].rearrange("p (b x) -> p b x", x=128)[:, :, 127]),
                       reads=[('l2', half)], writes=[('dcy', par, half)])

            def stageB(hd):
                par = hd % 2
                P = hgp[par]
                m3 = hd % 3
                ql, qb, khat, KH = (P[n] for n in ('ql', 'qb', 'khat', 'KH'))
                sog, VT = hg3[m3]['sog'], hg3[m3]['VT']

                def tk(n):
                    return (n, par)
                pt = ps[4][:, :].bitcast(BF16)
                for blk in range(NBLK):
                    OP('pe', lambda e, blk=blk: e.transpose(out=pt[:, blk * 128:(blk + 1) * 128],
                                                              in_=khat[:, blk * 128:(blk + 1) * 128], identity=ident[:, :]),
                         reads=[('khat', par, blk // 4, a_) for a_ in range(4)] + ['ident'], writes=[('ps', 4)])
                OP('dve', lambda e: e.tensor_copy(out=KT[:, :, :], in_=pt.rearrange("p (b m) -> p b m", m=128)),
                     reads=[('ps', 4)], writes=['KT'])
                for blk in range(NBLK):
                    b = 6 + blk // 4
                    for c in range(4):
                        m = 32 * (c + 1)
                        c0 = (blk % 4) * 128 + 32 * c
                        OP('pe', lambda e, blk=blk, c=c, m=m, c0=c0, b=b: e.matmul(
                            ps[b][0:m, c0:c0 + 32], lhsT=KH[:, blk, 128 * c:128 * c + m],
                            rhs=ql[:, blk * 128 + 32 * c:blk * 128 + 32 * c + 32], start=True, stop=True),
                            reads=[('KH', par, blk // 4, c, a_) for a_ in range(c + 1)] + [('ql', par, blk // 4)],
                            writes=[('ps', b)])
                for hb in range(2):
                    for c in range(4):
                        m = 32 * (c + 1)
                        OP('dve', lambda e, hb=hb, c=c, m=m: e.tensor_tensor(
                            out=AT[0:m, 4 * hb:4 * hb + 4, 32 * c:32 * c + 32],
                            in0=ps[6 + hb][0:m, :].rearrange("p (b x) -> p b x", x=128)[:, :, 32 * c:32 * c + 32],
                            in1=cmask[0:m, 32 * c:32 * c + 32].unsqueeze(1).to_broadcast([m, 4, 32]),
                            op=ALU.mult),
                            reads=[('ps', 6 + hb), 'consts'], writes=['AT'])
                for blk in range(NBLK):
                    ub = 4 + blk % 2
                    uc = (blk // 2) * 128
                    OP('pe', lambda e, blk=blk, ub=ub, uc=uc: e.matmul(
                        ps[ub][:, uc:uc + 128], lhsT=KT[:, blk, :], rhs=VT[:, blk, :],
                        start=True, stop=True),
                        reads=['KT', ('VT', m3)], writes=[('ps', ub)])
                OP('act', lambda e: e.activation(out=Sbf8[:, 0, :], in_=Sst[:, j, hd, :], func=AF.Copy),
                     reads=[('S', hd)], writes=[('Sbf8', 0)])
                for blk in range(NBLK):
                    ub = 4 + blk % 2
                    uc = (blk // 2) * 128
                    src = Sst[:, j, hd, :] if blk % 2 == 0 else S2[:, :]
                    dst = S2[:, :] if blk % 2 == 0 else Sst[:, j, hd, :]
                    stok = ('S', hd) if blk % 2 == 0 else 'S2'
                    dtok = 'S2' if blk % 2 == 0 else ('S', hd)
                    OP('dve', lambda e, blk=blk, ub=ub, uc=uc, src=src, dst=dst: e.scalar_tensor_tensor(
                        out=dst, in0=src, scalar=dcy[:, par, blk:blk + 1],
                        in1=ps[ub][:, uc:uc + 128], op0=ALU.mult, op1=ALU.add),
                        reads=[stok, ('dcy', par, blk // 4), ('ps', ub)], writes=[dtok])
                    if blk < NBLK - 1:
                        OP('act', lambda e, blk=blk, dst=dst: e.activation(out=Sbf8[:, blk + 1, :], in_=dst, func=AF.Copy),
                             reads=[dtok], writes=[('Sbf8', blk + 1)])
                for blk in range(NBLK):
                    bo = 6 + blk // 4
                    oc = (blk % 4) * 128
                    OP('pe', lambda e, blk=blk, bo=bo, oc=oc: e.matmul(
                        ps[bo][:, oc:oc + 128], lhsT=VT[:, blk, :], rhs=AT[:, blk, :], start=True, stop=False),
                        reads=[('VT', m3), 'AT'], writes=[('ps', bo)])
                    OP('pe', lambda e, blk=blk, bo=bo, oc=oc: e.matmul(
                        ps[bo][:, oc:oc + 128], lhsT=Sbf8[:, blk, :], rhs=qb[:, blk * 128:(blk + 1) * 128],
                        start=False, stop=True),
                        reads=[('Sbf8', blk), ('qb', par, blk // 4)], writes=[('ps', bo)])
                for half in range(2):
                    hs = H2[half]
                    OP('act', lambda e, half=half, hs=hs: e.activation(out=osq_b[:, hs], in_=ps[6 + half][:, :],
                                                                         func=AF.Square),
                         reads=[('ps', 6 + half)], writes=[('osq', half)])
                    OP('pe', lambda e, half=half, hs=hs: e.matmul(ps[4 + half][:, :], lhsT=ones_bf[:, :],
                                                                    rhs=osq_b[:, hs], start=True, stop=True),
                         reads=[('osq', half), 'ones'], writes=[('ps', 4 + half)])
                    OP('act', lambda e, half=half, hs=hs: e.activation(out=rs_b[:, hs], in_=ps[4 + half][:, :],
                                                                         func=AF.Ln, scale=1.0 / 128, bias=epscol[:, 0:1]),
                         reads=[('ps', 4 + half), 'eps'], writes=[('rs', half)])
                    OP('act', lambda e, hs=hs: e.activation(out=rs_b[:, hs], in_=rs_b[:, hs], func=AF.Exp, scale=-0.5),
                         reads=[('rs', half)], writes=[('rs', half)])
                    OP('dve', lambda e, half=half, hs=hs: e.tensor_tensor(out=on_b[:, hs], in0=ps[6 + half][:, :],
                                                                            in1=rs_b[:, hs], op=ALU.mult),
                         reads=[('ps', 6 + half), ('rs', half)], writes=[('on', half)])
                    OP('dve', lambda e, hs=hs: e.scalar_tensor_tensor(out=OG[:, hd, hs], in0=on_b[:, hs],
                                                                        scalar=ng[:, hd:hd + 1], in1=sog[:, hs],
                                                                        op0=ALU.mult, op1=ALU.mult),
                         reads=[('on', half), ('sog', m3), 'cols'], writes=[('og', hd)])

            def build(fn_, hd):
                del cur[:]
                fn_(hd)
                return list(cur)

            def emit(o):
                if o is None:
                    w_release()
                else:
                    S.op(o[0], o[1], reads=o[2], writes=o[3])

            def merge(lists):
                idx = [0] * len(lists)
                while True:
                    best = None
                    bt = None
                    for li, L in enumerate(lists):
                        if idx[li] >= len(L):
                            continue
                        o = L[idx[li]]
                        t = -1.0 if o is None else S.earliest(o[0], o[2], o[3])
                        if bt is None or t < bt:
                            bt, best = t, li
                    if best is None:
                        break
                    emit(lists[best][idx[best]])
                    idx[best] += 1

            merge([build(stageP, 0)])
            merge([build(stageE, 0), build(stageP, 1)])
            for hd in range(8):
                ls = [build(stageB, hd)]
                if hd + 1 < 8:
                    ls.append(build(stageE, hd + 1))
                if hd + 2 < 8:
                    ls.append(build(stageP, hd + 2))
                merge(ls)
            for dh in range(2):
                slot, wt = w_next()
                wv = slot[:, 0:4096].rearrange("p (u k m) -> p u k m", u=4, k=KC)
                for u in range(4):
                    dt = 4 * dh + u
                    bk = [next_bank(), next_bank()]
                    for kc in range(KC):
                        for half in range(2):
                            S.op('pe', lambda e, u=u, kc=kc, half=half, bk=bk: e.matmul(
                                ps[bk[half]][:, :], lhsT=wv[:, u, kc, :], rhs=OG[:, kc, half * 512:(half + 1) * 512],
                                start=(kc == 0), stop=(kc == KC - 1)),
                                reads=[wt, ('og', kc)], writes=[('ps', bk[half])])
                    for half in range(2):
                        hs = H2[half]
                        S.op('dve', lambda e, half=half, hs=hs, dt=dt, bk=bk: e.scalar_tensor_tensor(
                            out=xT[:, dt, hs], in0=ps[bk[half]][:, :], scalar=gcol[:, dt:dt + 1], in1=xT[:, dt, hs],
                            op0=ALU.mult, op1=ALU.add),
                            reads=[('ps', bk[half]), ('x', dt), 'dcol'], writes=[('x', dt)])
                w_release()

        xsrc = xT_d.rearrange("(k p) t -> p k t", p=128)
        odst = out_d.rearrange("(k p) t -> p k t", p=128)
        fg = cols[:, coff['fgain']:coff['fgain'] + 8]
        marks = []

        def mark(lbl):
            marks.append((lbl, S.cnt['pe']))
        for tile in range(ntile):
            t0 = tile * TT
            for kc in range(KC):
                S.op('sp', lambda e, t0=t0, kc=kc: e.dma_start(out=xT[:, kc, :], in_=xsrc[:, kc, t0:t0 + TT]),
                     writes=[('x', kc)], dma='xld%d' % kc)
            for i in range(depth):
                mark('t%d L%d norm1' % (tile, i))
                norm_phase(dcol[:, i, 0, :], dcol[:, i, 1, :])
                mark('t%d L%d %s' % (tile, i, kinds[i]))
                if kinds[i] == 'pool':
                    pool_phase(i, tile)
                else:
                    hgrn_phase(i, tile)
                mark('t%d L%d norm2' % (tile, i))
                norm_phase(dcol[:, i, 3, :], dcol[:, i, 4, :])
                mark('t%d L%d ffn' % (tile, i))
                ffn_phase(i)
            mark('t%d final' % tile)
            norm_phase(fg, None, final=True)
            for kc in range(KC):
                S.op('sp', lambda e, t0=t0, kc=kc: e.dma_start(out=odst[:, kc, t0:t0 + TT], in_=xT[:, kc, :]),
                     reads=[('x', kc)], writes=[('out', kc)], dma='ost%d' % kc)
        S.wait_all('sp', [('out', kc) for kc in range(KC)])
        mark('end')
        build_nc.stats = (S.n_ins, S.n_wait, dict(S.cnt))
        build_nc.marks = marks
        build_nc.sim = dict(S.eng_free)
    return nc


def make_consts():
    c = np.zeros((128, NCONST), np.float32)
    c[:, 0:128] = np.eye(128, dtype=np.float32)
    jj = np.arange(128)[:, None]
    ii = np.arange(128)[None, :]
    c[:, 128:256] = (jj <= ii).astype(np.float32)
    rc = np.zeros((4, HALO), np.float32)
    for g, w in enumerate(POOL_W):
        rc[g] = 1.0 / np.minimum(np.arange(HALO) + 1, w)
    c[:, 256:320] = rc.reshape(1, 64)
    return c


def colfmt(v):
    v = np.asarray(v, np.float32)
    return np.ascontiguousarray(v.reshape(-1, 128).T)


def host_layout(inp, depth):
    coff, ncols = cols_layout(depth)
    B = inp['x'].shape[0]
    chunks = pass_chunks(depth)
    ws = np.concatenate([host_chunk(k, i, idx, inp) for (k, i, idx, n) in chunks], axis=1)
    ws = np.ascontiguousarray(ws, dtype=np.float32)
    al = []
    for i in range(depth):
        w = inp['ada_w'][i].reshape(8, 128, 12, 512)
        al.append(w.transpose(1, 2, 0, 3).reshape(128, 12 * 4096))
    astream = np.ascontiguousarray(np.concatenate(al, axis=1), dtype=np.float32)
    consts = make_consts()
    maps = []
    for b in range(B):
        cols = np.zeros((128, ncols), np.float32)
        cols[:, coff['c']:coff['c'] + 8] = colfmt(inp['c'][b])
        for i in range(depth):
            cols[:, coff['gmix'] + 8 * i: coff['gmix'] + 8 * i + 8] = colfmt(inp['norm_mix_gain'][i])
            cols[:, coff['gffn'] + 8 * i: coff['gffn'] + 8 * i + 8] = colfmt(inp['norm_ffn_gain'][i])
        for j in range((depth + 1) // 2):
            cols[:, coff['pscale'] + 8 * j: coff['pscale'] + 8 * j + 8] = colfmt(inp['pool_scale'][j])
        for j in range(depth // 2):
            cols[:, coff['hng'] + 8 * j: coff['hng'] + 8 * j + 8] = colfmt(inp['hgrn_norm_gain'][j])
        for j in range(min(2, inp['hgrn_lb_logits'].shape[0])):
            cols[:, coff['lbl'] + 8 * j: coff['lbl'] + 8 * j + 8] = colfmt(inp['hgrn_lb_logits'][j])
        cols[:, coff['fgain']:coff['fgain'] + 8] = colfmt(inp['final_gain'])
        for i in range(depth):
            cols[:, coff['adab'] + 48 * i: coff['adab'] + 48 * i + 48] = colfmt(inp['ada_b'][i])
        maps.append({
            "xT": np.ascontiguousarray(inp['x'][b].T),
            "wstream": ws, "astream": astream, "cols": cols, "consts": consts,
        })
    return maps


def run(inp, depth, T, trace=False):
    B = inp['x'].shape[0]
    nc = build_nc(T, depth)
    maps = host_layout(inp, depth)
    res = run_bass_kernel_spmd(nc, maps, core_ids=list(range(B)), trace=trace)
    out = np.stack([np.ascontiguousarray(r["outT"].T) for r in res.results], axis=0)
    return out.astype(np.float32), res


def kernel(**inputs):
    inp = {k: np.asarray(v) for k, v in inputs.items()}
    out, _ = run(inp, 4, inp['x'].shape[1])
    return out
```

```python
import contextlib
import numpy as np
import concourse.bass as bass
import concourse.mybir as mybir
from concourse.bass_utils import run_bass_kernel_spmd

F32 = mybir.dt.float32
BF16 = mybir.dt.bfloat16
AF = mybir.ActivationFunctionType
ALU = mybir.AluOpType

D = 1024
KC = 8
DFF = 2816
NF = 22
TT = 1024
HALO = 16
NBLK = TT // 128
EPS = 1e-6
NB_RING = 3
SLOT = 4096
POOL_W = (2, 4, 8, 16)


class Sched:
    def __init__(self, nc, stack):
        self.nc = nc
        self.stack = stack
        self.engs = {'pe': nc.tensor, 'act': nc.scalar, 'dve': nc.vector,
                     'pool': nc.gpsimd, 'sp': nc.sync}
        self.semh = {}
        self.cnt = {}
        for e in self.engs:
            self.semh[e] = stack.enter_context(nc.semaphore("prog_" + e))
            self.cnt[e] = 0
        self.seen = {e: {} for e in self.engs}
        self.last_w = {}
        self.readers = {}
        self.n_wait = 0
        self.n_ins = 0
        self.eng_free = {e: 0.0 for e in self.engs}
        self.tok_w = {}
        self.tok_r = {}

    class _Rec:
        def __getattr__(self, name):
            return lambda *a, **k: (name, a, k)

    @staticmethod
    def _fsz(ap):
        n = 1
        for d in ap.shape[1:]:
            n *= int(d)
        return n

    def est_dur(self, eng, fn, dma):
        if dma is not None:
            return 6.0
        try:
            name, a, k = fn(Sched._Rec())
            if name == 'matmul':
                return 0.07 + Sched._fsz(k['rhs']) / 1950.0
            if name == 'transpose':
                return 0.13
            out = k.get('out', a[0] if a else None)
            n = Sched._fsz(out)
            if name == 'activation':
                return 0.25 + n / 1400.0
            if name == 'tensor_tensor_scan':
                return 0.12 + 2.0 * n / 960.0
            return 0.1 + n / 960.0
        except Exception:
            return 0.5

    def earliest(self, eng, reads, writes):
        t = self.eng_free[eng]
        for r in reads:
            v = self.tok_w.get(r, 0.0)
            if v > t:
                t = v
        for w in writes:
            v = self.tok_w.get(w, 0.0)
            if v > t:
                t = v
            v = self.tok_r.get(w, 0.0)
            if v > t:
                t = v
        return t

    def op(self, eng, fn, reads=(), writes=(), dma=None):
        dur = self.est_dur(eng, fn, dma)
        st = self.earliest(eng, reads, writes)
        fin = st + dur + 0.12
        self.eng_free[eng] = (st + 0.1) if dma is not None else (st + dur)
        for t in reads:
            if fin > self.tok_r.get(t, 0.0):
                self.tok_r[t] = fin
        for t in writes:
            self.tok_w[t] = fin
            self.tok_r[t] = 0.0
        need = {}
        own_ok = (dma is None)
        for t in reads:
            w = self.last_w.get(t)
            if w is not None:
                k, v = w
                if not (own_ok and k == eng and eng == 'pe'):
                    if v > need.get(k, 0):
                        need[k] = v
            if type(t) is tuple and t[0] == 'ps':
                rd = self.readers.get(t)
                if rd:
                    for k, v in rd.items():
                        if k != eng and v > need.get(k, 0):
                            need[k] = v
        for t in writes:
            w = self.last_w.get(t)
            if w is not None:
                k, v = w
                if not (own_ok and k == eng and eng == 'pe'):
                    if v > need.get(k, 0):
                        need[k] = v
            rd = self.readers.get(t)
            if rd:
                for k, v in rd.items():
                    if own_ok and k == eng and eng == 'pe':
                        continue
                    if v > need.get(k, 0):
                        need[k] = v
        e = self.engs[eng]
        seen = self.seen[eng]
        for k, v in need.items():
            if seen.get(k, 0) >= v:
                continue
            e.wait_ge(self.semh[k], v)
            self.n_wait += 1
            seen[k] = v
        ins = fn(e)
        self.n_ins += 1
        if dma is None:
            self.cnt[eng] += 1
            ins.then_inc(self.semh[eng], 1)
            me = (eng, self.cnt[eng])
        else:
            if dma not in self.semh:
                self.semh[dma] = self.stack.enter_context(self.nc.semaphore("dma_" + dma))
                self.cnt[dma] = 0
            self.cnt[dma] += 16
            ins.then_inc(self.semh[dma], 16)
            me = (dma, self.cnt[dma])
        for t in reads:
            rd = self.readers.setdefault(t, {})
            if me[1] > rd.get(me[0], 0):
                rd[me[0]] = me[1]
        for t in writes:
            self.last_w[t] = me
            self.readers[t] = {}
        return me

    def wait_all(self, eng, toks):
        e = self.engs[eng]
        for t in toks:
            w = self.last_w.get(t)
            if w is None:
                continue
            if self.seen[eng].get(w[0], 0) >= w[1]:
                continue
            e.wait_ge(self.semh[w[0]], w[1])
            self.seen[eng][w[0]] = w[1]


def layer_kinds(depth):
    return ['pool' if i % 2 == 0 else 'hgrn' for i in range(depth)]


def pass_chunks(depth):
    out = []
    for i, kind in enumerate(layer_kinds(depth)):
        if kind == 'pool':
            out.append(('pool', i, 0, 2048))
        else:
            for hd in range(8):
                out.append(('hin', i, hd, 4096))
            for dh in range(2):
                out.append(('hout', i, dh, 4096))
        for pi in range(NF // 2):
            out.append(('fin', i, pi, 4096))
        for dt in range(KC):
            out.append(('fout', i, dt, NF * 128))
    return out


def host_chunk(kind, i, idx, inp):
    j = i // 2
    if kind == 'pool':
        w = inp['pool_w'][j]
        a = w.reshape(4, 2, 128, 256).transpose(2, 0, 1, 3)
        return a.reshape(128, 2048)
    if kind == 'hin':
        w = inp['hgrn_w_in'][j]
        a = w.reshape(8, 128, 4, 8, 128)[:, :, :, idx, :]
        return a.transpose(1, 0, 2, 3).reshape(128, 4096)
    if kind == 'hout':
        w = inp['hgrn_w_out'][j]
        a = w.reshape(8, 128, 2, 4, 128)[:, :, idx, :, :]
        return a.transpose(1, 2, 0, 3).reshape(128, 4096)
    if kind == 'fin':
        w = inp['ffn_w_in'][i]
        wr = w.reshape(8, 128, 2, NF, 128)
        a = wr[:, :, :, 2 * idx:2 * idx + 2, :]
        return a.transpose(1, 3, 0, 2, 4).reshape(128, 4096)
    if kind == 'fout':
        w = inp['ffn_w_out'][i]
        a = w.reshape(NF, 128, 8, 128)[:, :, idx, :]
        return a.transpose(1, 0, 2).reshape(128, NF * 128)
    raise ValueError(kind)


def cols_layout(depth):
    npool = (depth + 1) // 2
    nh = depth // 2
    off = {}
    o = 0
    for name, n in (('c', 8), ('gmix', depth * 8), ('gffn', depth * 8), ('pscale', npool * 8),
                    ('lbl', 2 * 8 * max(nh, 1)), ('hng', max(nh, 1) * 8), ('fgain', 8),
                    ('adab', depth * 48)):
        off[name] = o
        o += n
    return off, o


NCONST = 128 + 128 + 64


def build_nc(T, depth):
    ntile = T // TT
    kinds = layer_kinds(depth)
    coff, ncols = cols_layout(depth)
    chunks = pass_chunks(depth)
    pass_cols = sum(c[3] for c in chunks)
    a_cols = depth * 12 * 4096

    nc = bass.Bass("TRN2", target_bir_lowering=False)
    xT_d = nc.dram_tensor("xT", [D, T], F32, kind="ExternalInput").ap()
    ws_d = nc.dram_tensor("wstream", [128, pass_cols], F32, kind="ExternalInput").ap()
    as_d = nc.dram_tensor("astream", [128, a_cols], F32, kind="ExternalInput").ap()
    cols_d = nc.dram_tensor("cols", [128, ncols], F32, kind="ExternalInput").ap()
    const_d = nc.dram_tensor("consts", [128, NCONST], F32, kind="ExternalInput").ap()
    out_d = nc.dram_tensor("outT", [D, T], F32, kind="ExternalOutput").ap()

    with contextlib.ExitStack() as st:
        S = Sched(nc, st)

        def sb(name, shape, dt):
            return st.enter_context(nc.sbuf_tensor(name, shape, dt))

        xT = sb("xT_sb", [128, KC, TT], F32)
        hT = sb("hT_sb", [128, KC, HALO + TT], BF16)
        sq = sb("sq_sb", [128, KC, TT], BF16)
        rstd = sb("rstd_sb", [128, TT], F32)
        ntmp = [sb("ntmp%d" % i, [128, TT], F32) for i in range(2)]
        ring = [sb("ring%d" % i, [128, SLOT], BF16) for i in range(NB_RING)]
        ARENA_B = 65536
        arena = sb("arena", [128, ARENA_B // 2], BF16)
        sg = [sb("sg%d" % i, [128, TT], BF16) for i in range(2)]
        cols = sb("cols_sb", [128, ncols], F32)
        consts = sb("consts_sb", [128, NCONST], F32)
        ident = sb("ident_bf", [128, 128], BF16)
        ones_bf = sb("ones_bf", [128, 128], BF16)
        mask32 = sb("mask32", [128, 512], BF16)
        mask128 = sb("mask128", [128, 512], BF16)
        epscol = sb("epscol", [128, 1], F32)
        one11 = sb("one11", [1, 1], F32)
        cact = sb("cact", [128, KC], BF16)
        rowbuf = [sb("rowbuf%d" % i, [1, 512], F32) for i in range(2)]
        modc = sb("modc", [128, depth, 48], F32)
        modraw = sb("modraw", [128, depth, 48], F32)
        dcol = sb("dcol", [128, depth, 6, KC], F32)
        nh = max(depth // 2, 1)
        npool = (depth + 1) // 2
        lbc = sb("lbc", [128, nh, 2, 8], F32)
        Sst = sb("S_state", [128, nh, 8, 128], F32)
        Sbf8 = sb("S_bf8", [128, NBLK, 128], BF16)
        S2 = sb("S2", [128, 128], F32)
        halo = sb("halo", [128, npool, KC, HALO], BF16)
        OG = sb("og_sb", [128, KC, TT], BF16)
        tmp16 = sb("tmp16", [128, 2, HALO], F32)

        ps = [st.enter_context(nc.psum_tensor("psb%d" % i, [128, 512], F32)) for i in range(8)]

        def aview(byte_off, shape, dt):
            esz = 4 if dt == F32 else 2
            n = int(np.prod(shape[1:]))
            a = arena[:, byte_off // 2: byte_off // 2 + n * esz // 2]
            if dt == F32:
                a = a.bitcast(F32)
            if len(shape) == 3:
                a = a.rearrange("p (a b) -> p a b", b=shape[2])
            elif len(shape) == 4:
                a = a.rearrange("p (a b c) -> p a b c", b=shape[2], c=shape[3])
            return a

        act_v = aview(0, [128, NF, TT], BF16)
        sA = aview(0, [128, 2, HALO + TT], F32)
        sB = aview(8320, [128, 2, HALO + TT], F32)
        def rawview(base2d, byte_off, shape, dt):
            esz = 4 if dt == F32 else 2
            n = int(np.prod(shape[1:]))
            a = base2d[:, byte_off // 2: byte_off // 2 + n * esz // 2]
            if dt == F32:
                a = a.bitcast(F32)
            if len(shape) == 3:
                a = a.rearrange("p (a b) -> p a b", b=shape[2])
            return a

        hg = {}
        o = 0
        for nm in ('e0', 'e1', 'l1', 'l2', 'kk'):
            hg[nm] = aview(o, [128, TT], F32)
            o += 4096
        hg['bl'] = ntmp[0]
        hg['bb'] = ntmp[1]
        hgp = []
        for par in range(2):
            d = {}
            for nm in ('ql', 'qb', 'khat', 'qsb'):
                d[nm] = aview(o, [128, TT], BF16)
                o += 2048
            d['KH'] = aview(o, [128, NBLK, 512], BF16); o += 8192
            d['e'] = hg['e%d' % par]
            hgp.append(d)
        hg3 = []
        for m3 in range(3):
            d = {}
            d['sog'] = aview(o, [128, TT], BF16); o += 2048
            d['VT'] = aview(o, [128, NBLK, 128], BF16); o += 2048
            hg3.append(d)
        assert o <= ARENA_B, o
        sqflat = sq[:, :, :].rearrange("p a b -> p (a b)")
        KT = rawview(sqflat, 0, [128, NBLK, 128], BF16)
        AT = rawview(sqflat, 2048, [128, NBLK, 128], BF16)
        osq_b = rawview(sqflat, 4096, [128, TT], BF16)
        rs_b = rawview(sqflat, 6144, [128, TT], F32)
        on_b = rawview(sqflat, 10240, [128, TT], F32)
        dcy = sb("dcy", [128, 2, NBLK], F32)
        Gs = sb("Gs", [128, NBLK, 6], F32)

        ident_f = consts[:, 0:128]
        cmask = consts[:, 128:256]
        rcnt = consts[:, 256:320].rearrange("p (g t) -> p g t", t=HALO)

        stream = []

        def ada_src(i, nb):
            o0 = (i * 12 + nb) * 4096
            return (as_d[:, o0:o0 + 4096], 4096)
        for nb in range(12):
            stream.append(ada_src(0, nb))
        for t in range(ntile):
            o0 = 0
            for (_k, _i, _x, n) in chunks:
                stream.append((ws_d[:, o0:o0 + n], n))
                o0 += n
                if t == 0 and _k == 'fin' and _i + 1 < depth:
                    stream.append(ada_src(_i + 1, _x))
                    if _x == NF // 2 - 1:
                        stream.append(ada_src(_i + 1, 11))
        wstate = {'issued': 0, 'next': 0}

        def w_issue():
            k = wstate['issued']
            if k >= len(stream):
                return
            src, n = stream[k]
            s = k % NB_RING
            S.op('pool', lambda e: e.dma_start(out=ring[s][:, 0:n], in_=src),
                 writes=[('w', s)], dma='ring%d' % s)
            wstate['issued'] += 1

        def w_next():
            k = wstate['next']
            while wstate['issued'] <= k:
                w_issue()
            wstate['next'] += 1
            s = k % NB_RING
            return ring[s], ('w', s)

        def w_release():
            w_issue()

        psrot = {'i': 0}

        def next_bank():
            b = psrot['i'] % 8
            psrot['i'] += 1
            return b

        S.op('sp', lambda e: e.dma_start(out=cols[:], in_=cols_d), writes=['cols'], dma='c0')
        S.op('sp', lambda e: e.dma_start(out=consts[:], in_=const_d), writes=['consts'], dma='c1')
        for _ in range(NB_RING):
            w_issue()
        S.op('dve', lambda e: e.memset(ones_bf[:], 1.0), writes=['ones'])
        S.op('dve', lambda e: e.memset(epscol[:], EPS), writes=['eps'])
        S.op('dve', lambda e: e.memset(one11[:], 1.0), writes=['one11'])
        S.op('dve', lambda e: e.memset(mask32[:], 1.0), writes=['m32'])
        S.op('dve', lambda e: e.memset(mask32[:].rearrange("p (c j) -> p c j", j=32)[:, :, 0:1], 0.0),
             writes=['m32'])
        S.op('dve', lambda e: e.memset(mask128[:], 1.0), writes=['m128'])
        S.op('dve', lambda e: e.memset(mask128[:].rearrange("p (c j) -> p c j", j=128)[:, :, 0:1], 0.0),
             writes=['m128'])
        S.op('dve', lambda e: e.memset(halo[:], 0.0), writes=[('halo', jj) for jj in range(npool)])
        S.op('dve', lambda e: e.memset(Sst[:], 0.0), writes=[('S', hh) for hh in range(8)])
        S.op('dve', lambda e: e.tensor_copy(out=ident[:], in_=ident_f), reads=['consts'], writes=['ident'])
        S.op('act', lambda e: e.activation(out=cact[:], in_=cols[:, coff['c']:coff['c'] + 8], func=AF.Silu),
             reads=['cols'], writes=['cact'])

        def ada_chunk(i, nb):
            slot, wt = w_next()
            wv = slot[:, 0:4096].rearrange("p (k n) -> p k n", n=512)
            b = next_bank()
            b2 = next_bank()
            rb = rowbuf[nb % 2]
            rbt = ('rowbuf', nb % 2)
            for kc in range(KC):
                S.op('pe', lambda e, kc=kc: e.matmul(ps[b][0:1, :], lhsT=cact[:, kc:kc + 1], rhs=wv[:, kc, :],
                                                     start=(kc == 0), stop=(kc == KC - 1)),
                     reads=[wt, 'cact'], writes=[('ps', b)])
            S.op('dve', lambda e: e.tensor_copy(out=rb[0:1, :], in_=ps[b][0:1, :]),
                 reads=[('ps', b)], writes=[rbt])
            for jj in range(4):
                S.op('pe', lambda e, jj=jj: e.matmul(
                    ps[b2][:, jj:jj + 1], lhsT=rb[0:1, jj * 128:(jj + 1) * 128],
                    rhs=one11[0:1, 0:1], start=True, stop=True),
                    reads=[rbt, 'one11'], writes=[('ps', b2)])
            S.op('dve', lambda e: e.tensor_copy(out=modraw[:, i, nb * 4:nb * 4 + 4], in_=ps[b2][:, 0:4]),
                 reads=[('ps', b2)], writes=['modraw'])
            w_release()

        def ada_finish(i):
            ab = cols[:, coff['adab'] + i * 48: coff['adab'] + i * 48 + 48]
            S.op('dve', lambda e: e.tensor_tensor(out=modc[:, i, :], in0=modraw[:, i, :], in1=ab, op=ALU.add),
                 reads=['modraw', 'cols'], writes=['modc'])
            gm = cols[:, coff['gmix'] + i * 8: coff['gmix'] + i * 8 + 8]
            gf = cols[:, coff['gffn'] + i * 8: coff['gffn'] + i * 8 + 8]
            S.op('dve', lambda e: e.scalar_tensor_tensor(out=dcol[:, i, 0, :], in0=modc[:, i, 8:16], scalar=1.0,
                                                         in1=gm, op0=ALU.add, op1=ALU.mult),
                 reads=['modc', 'cols'], writes=['dcol'])
            S.op('dve', lambda e: e.tensor_copy(out=dcol[:, i, 1, :], in_=modc[:, i, 0:8]),
                 reads=['modc'], writes=['dcol'])
            if kinds[i] == 'pool':
                psc = cols[:, coff['pscale'] + (i // 2) * 8: coff['pscale'] + (i // 2) * 8 + 8]
                S.op('dve', lambda e: e.tensor_tensor(out=dcol[:, i, 2, :], in0=modc[:, i, 16:24], in1=psc,
                                                      op=ALU.mult), reads=['modc', 'cols'], writes=['dcol'])
            else:
                S.op('dve', lambda e: e.tensor_copy(out=dcol[:, i, 2, :], in_=modc[:, i, 16:24]),
                     reads=['modc'], writes=['dcol'])
            S.op('dve', lambda e: e.scalar_tensor_tensor(out=dcol[:, i, 3, :], in0=modc[:, i, 32:40], scalar=1.0,
                                                         in1=gf, op0=ALU.add, op1=ALU.mult),
                 reads=['modc', 'cols'], writes=['dcol'])
            S.op('dve', lambda e: e.tensor_copy(out=dcol[:, i, 4, :], in_=modc[:, i, 24:32]),
                 reads=['modc'], writes=['dcol'])
            S.op('dve', lambda e: e.tensor_copy(out=dcol[:, i, 5, :], in_=modc[:, i, 40:48]),
                 reads=['modc'], writes=['dcol'])
        for nb in range(12):
            ada_chunk(0, nb)
        ada_finish(0)
        for j in range(depth // 2):
            if j == 0:
                S.op('dve', lambda e: e.memset(lbc[:, 0, 0, :], 0.0), writes=['lbc'])
                S.op('dve', lambda e: e.memset(lbc[:, 0, 1, :], 1.0), writes=['lbc'])
            else:
                l0 = cols[:, coff['lbl']: coff['lbl'] + 8]
                l1 = cols[:, coff['lbl'] + 8: coff['lbl'] + 16]
                S.op('dve', lambda e: e.tensor_tensor(out=lbc[:, j, 0, :], in0=l0, in1=l1, op=ALU.subtract),
                     reads=['cols'], writes=['lbc'])
                S.op('act', lambda e: e.activation(out=lbc[:, j, 0, :], in_=lbc[:, j, 0, :], func=AF.Exp),
                     reads=['lbc'], writes=['lbc'])
                S.op('dve', lambda e: e.tensor_scalar_add(out=lbc[:, j, 0, :], in0=lbc[:, j, 0, :], scalar1=1.0),
                     reads=['lbc'], writes=['lbc'])
                S.op('dve', lambda e: e.reciprocal(out=lbc[:, j, 0, :], in_=lbc[:, j, 0, :]),
                     reads=['lbc'], writes=['lbc'])
                S.op('dve', lambda e: e.tensor_scalar(out=lbc[:, j, 1, :], in0=lbc[:, j, 0, :], scalar1=-1.0,
                                                      scalar2=1.0, op0=ALU.mult, op1=ALU.add),
                     reads=['lbc'], writes=['lbc'])

        XTOK = [('x', kc) for kc in range(KC)]
        HTOK = [('h', kc) for kc in range(KC)]

        def norm_phase(acol, bcol, final=False):
            for kc in range(KC):
                S.op('act', lambda e, kc=kc: e.activation(out=sq[:, kc, :], in_=xT[:, kc, :], func=AF.Square),
                     reads=[('x', kc)], writes=[('sq', kc)])
            bk = [next_bank(), next_bank()]
            for half in range(2):
                for kc in range(KC):
                    S.op('pe', lambda e, kc=kc, half=half: e.matmul(
                        ps[bk[half]][:, :], lhsT=ones_bf[:, :], rhs=sq[:, kc, half * 512:(half + 1) * 512],
                        start=(kc == 0), stop=(kc == KC - 1)),
                        reads=[('sq', kc), 'ones'], writes=[('ps', bk[half])])
            for half in range(2):
                hs = slice(half * 512, (half + 1) * 512)
                S.op('act', lambda e, half=half, hs=hs: e.activation(out=rstd[:, hs], in_=ps[bk[half]][:, :],
                                                                     func=AF.Ln, scale=1.0 / D, bias=epscol[:, 0:1]),
                     reads=[('ps', bk[half]), 'eps'], writes=[('rstd', half)])
                S.op('act', lambda e, hs=hs: e.activation(out=rstd[:, hs], in_=rstd[:, hs], func=AF.Exp, scale=-0.5),
                     reads=[('rstd', half)], writes=[('rstd', half)])
            for kc in range(KC):
                if final:
                    S.op('dve', lambda e, kc=kc: e.scalar_tensor_tensor(
                        out=xT[:, kc, :], in0=xT[:, kc, :], scalar=acol[:, kc:kc + 1], in1=rstd[:, :],
                        op0=ALU.mult, op1=ALU.mult),
                        reads=[('x', kc), ('rstd', 0), ('rstd', 1), 'dcol', 'cols'], writes=[('x', kc)])
                else:
                    nt = ntmp[kc % 2]
                    S.op('dve', lambda e, kc=kc, nt=nt: e.scalar_tensor_tensor(
                        out=nt[:, :], in0=xT[:, kc, :], scalar=acol[:, kc:kc + 1], in1=rstd[:, :],
                        op0=ALU.mult, op1=ALU.mult),
                        reads=[('x', kc), ('rstd', 0), ('rstd', 1), 'dcol'], writes=[('ntmp', kc % 2)])
                    S.op('act', lambda e, kc=kc, nt=nt: e.activation(
                        out=hT[:, kc, HALO:HALO + TT], in_=nt[:, :], func=AF.Identity, bias=bcol[:, kc:kc + 1],
                        scale=1.0),
                        reads=[('ntmp', kc % 2), 'dcol'], writes=[('h', kc)])

        def ffn_phase(i, tile=1):
            gcol = dcol[:, i, 5, :]
            do_ada = (tile == 0 and i + 1 < depth)
            for pi in range(NF // 2):
                slot, wt = w_next()
                wv = slot[:, 0:4096].rearrange("p (u k w m) -> p u k w m", u=2, k=KC, w=2)
                for u in range(2):
                    ft = 2 * pi + u
                    bg = [next_bank(), next_bank()]
                    bu = [next_bank(), next_bank()]
                    for which, bks in ((0, bg), (1, bu)):
                        for kc in range(KC):
                            for half in range(2):
                                S.op('pe', lambda e, kc=kc, half=half, which=which, bks=bks, u=u: e.matmul(
                                    ps[bks[half]][:, :], lhsT=wv[:, u, kc, which, :],
                                    rhs=hT[:, kc, HALO + half * 512:HALO + (half + 1) * 512],
                                    start=(kc == 0), stop=(kc == KC - 1)),
                                    reads=[wt, ('h', kc)], writes=[('ps', bks[half])])
                    sgb = sg[ft % 2]
                    for half in range(2):
                        hs = slice(half * 512, (half + 1) * 512)
                        S.op('act', lambda e, half=half, hs=hs, sgb=sgb, bg=bg: e.activation(
                            out=sgb[:, hs], in_=ps[bg[half]][:, :], func=AF.Silu),
                            reads=[('ps', bg[half])], writes=[('sg', ft % 2, half)])
                        S.op('dve', lambda e, half=half, hs=hs, sgb=sgb, bu=bu, ft=ft: e.tensor_tensor(
                            out=act_v[:, ft, hs], in0=ps[bu[half]][:, :], in1=sgb[:, hs], op=ALU.mult),
                            reads=[('ps', bu[half]), ('sg', ft % 2, half)], writes=[('act', ft)])
                w_release()
                if do_ada:
                    ada_chunk(i + 1, pi)
                    if pi == NF // 2 - 1:
                        ada_chunk(i + 1, 11)
                        ada_finish(i + 1)
            for dt in range(KC):
                slot, wt = w_next()
                wv = slot[:, 0:NF * 128].rearrange("p (f m) -> p f m", m=128)
                bk = [next_bank(), next_bank()]
                for fk in range(NF):
                    for half in range(2):
                        S.op('pe', lambda e, fk=fk, half=half: e.matmul(
                            ps[bk[half]][:, :], lhsT=wv[:, fk, :], rhs=act_v[:, fk, half * 512:(half + 1) * 512],
                            start=(fk == 0), stop=(fk == NF - 1)),
                            reads=[wt, ('act', fk)], writes=[('ps', bk[half])])
                for half in range(2):
                    hs = slice(half * 512, (half + 1) * 512)
                    S.op('dve', lambda e, half=half, hs=hs, dt=dt: e.scalar_tensor_tensor(
                        out=xT[:, dt, hs], in0=ps[bk[half]][:, :], scalar=gcol[:, dt:dt + 1], in1=xT[:, dt, hs],
                        op0=ALU.mult, op1=ALU.add),
                        reads=[('ps', bk[half]), ('x', dt), 'dcol'], writes=[('x', dt)])
                w_release()

        def pool_phase(i, tile):
            j = i // 2
            gcol = dcol[:, i, 2, :]
            S.op('dve', lambda e: e.tensor_copy(out=hT[:, :, 0:HALO], in_=halo[:, j, :, :]),
                 reads=[('halo', j)], writes=['hh'])
            dd = sq
            for g in range(4):
                w = POOL_W[g]
                c0 = 2 * g
                hv = hT[:, c0:c0 + 2, :]
                rtok = [('h', c0), ('h', c0 + 1), 'hh']
                L = HALO + TT
                S.op('dve', lambda e, hv=hv: e.tensor_tensor(out=sA[:, :, 1:L], in0=hv[:, :, 1:L], in1=hv[:, :, 0:L - 1],
                                                             op=ALU.add), reads=rtok, writes=['sA'])
                cur, curt = sA, 'sA'
                oth, otht = sB, 'sB'
                sh = 1
                lo = 1
                for lev in range(1, g + 1):
                    sh = 2 ** lev
                    lo2 = lo + sh
                    S.op('dve', lambda e, cur=cur, oth=oth, lo2=lo2, sh=sh: e.tensor_tensor(
                        out=oth[:, :, lo2:L], in0=cur[:, :, lo2:L], in1=cur[:, :, lo2 - sh:L - sh], op=ALU.add),
                        reads=[curt], writes=[otht])
                    cur, curt, oth, otht = oth, otht, cur, curt
                    lo = lo2
                for cc in range(2):
                    S.op('dve', lambda e, cur=cur, cc=cc, w=w, c0=c0: e.scalar_tensor_tensor(
                        out=dd[:, c0 + cc, :], in0=cur[:, cc, HALO:L], scalar=1.0 / w, in1=hT[:, c0 + cc, HALO:L],
                        op0=ALU.mult, op1=ALU.subtract),
                        reads=[curt, ('h', c0 + cc)], writes=[('sq', c0 + cc)])
                if tile == 0:
                    for cc in range(2):
                        S.op('dve', lambda e, cur=cur, cc=cc, g=g: e.tensor_tensor(
                            out=tmp16[:, cc, :], in0=cur[:, cc, HALO:2 * HALO], in1=rcnt[:, g, :], op=ALU.mult),
                            reads=[curt, 'consts'], writes=[('t16', cc)])
                        S.op('dve', lambda e, cc=cc, c0=c0: e.tensor_tensor(
                            out=dd[:, c0 + cc, 0:HALO], in0=tmp16[:, cc, :], in1=hT[:, c0 + cc, HALO:2 * HALO],
                            op=ALU.subtract),
                            reads=[('t16', cc), ('h', c0 + cc)], writes=[('sq', c0 + cc)])
            S.op('dve', lambda e: e.tensor_copy(out=halo[:, j, :, :], in_=hT[:, :, TT:TT + HALO]),
                 reads=HTOK + ['hh'], writes=[('halo', j)])
            slot, wt = w_next()
            wv = slot[:, 0:2048].rearrange("p (g c e) -> p g c e", g=4, c=2)
            for g in range(4):
                for ec in range(2):
                    bk = [next_bank(), next_bank()]
                    dt = 2 * g + ec
                    for cc in range(2):
                        for half in range(2):
                            S.op('pe', lambda e, g=g, ec=ec, cc=cc, half=half, bk=bk: e.matmul(
                                ps[bk[half]][:, :], lhsT=wv[:, g, cc, ec * 128:(ec + 1) * 128],
                                rhs=dd[:, 2 * g + cc, half * 512:(half + 1) * 512],
                                start=(cc == 0), stop=(cc == 1)),
                                reads=[wt, ('sq', 2 * g + cc)], writes=[('ps', bk[half])])
                    for half in range(2):
                        hs = slice(half * 512, (half + 1) * 512)
                        S.op('dve', lambda e, half=half, hs=hs, dt=dt, bk=bk: e.scalar_tensor_tensor(
                            out=xT[:, dt, hs], in0=ps[bk[half]][:, :], scalar=gcol[:, dt:dt + 1], in1=xT[:, dt, hs],
                            op0=ALU.mult, op1=ALU.add),
                            reads=[('ps', bk[half]), ('x', dt), 'dcol'], writes=[('x', dt)])
            w_release()

        H2 = [slice(0, 512), slice(512, 1024)]

        def hgrn_phase(i, tile):
            j = i // 2
            gcol = dcol[:, i, 2, :]
            ng = cols[:, coff['hng'] + j * 8: coff['hng'] + j * 8 + 8]
            l1, l2, bl, bb, kk = (hg[n] for n in ('l1', 'l2', 'bl', 'bb', 'kk'))
            ex = l2
            S.op('dve', lambda e: e.memset(AT[:], 0.0), writes=['AT'])
            cur = []

            def OP(eng, fn, reads=(), writes=()):
                cur.append((eng, fn, list(reads), list(writes)))

            def stageP(hd):
                par = hd % 2
                m3 = hd % 3
                e_ = hgp[par]['e']
                qsb = hgp[par]['qsb']
                sog, VT = hg3[m3]['sog'], hg3[m3]['VT']
                slot, wt = w_next()
                wv = slot[:, 0:4096].rearrange("p (k s m) -> p k s m", k=KC, s=4)

                def proj(sidx, banks):
                    for kc in range(KC):
                        for half in range(2):
                            OP('pe', lambda e, kc=kc, half=half: e.matmul(
                                ps[banks[half]][:, :], lhsT=wv[:, kc, sidx, :],
                                rhs=hT[:, kc, HALO + half * 512:HALO + (half + 1) * 512],
                                start=(kc == 0), stop=(kc == KC - 1)),
                                reads=[wt, ('h', kc)], writes=[('ps', banks[half])])
                proj(1, [0, 1])
                for half in range(2):
                    hs = H2[half]
                    OP('act', lambda e, half=half, hs=hs: e.activation(out=e_[:, hs], in_=ps[half][:, :], func=AF.Exp,
                                                                       scale=-1.0),
                       reads=[('ps', half)], writes=[('e', par, half)])
                proj(3, [2, 3])
                for half in range(2):
                    hs = H2[half]
                    OP('act', lambda e, half=half, hs=hs: e.activation(out=sog[:, hs], in_=ps[2 + half][:, :],
                                                                       func=AF.Silu),
                       reads=[('ps', 2 + half)], writes=[('sog', m3)])
                proj(0, [0, 1])
                for half in range(2):
                    hs = H2[half]
                    OP('act', lambda e, half=half, hs=hs: e.activation(out=qsb[:, hs], in_=ps[half][:, :], func=AF.Copy),
                       reads=[('ps', half)], writes=[('qsb', par, half)])
                for blk in range(NBLK):
                    b = 2 + blk // 4
                    for kc in range(KC):
                        OP('pe', lambda e, blk=blk, kc=kc, b=b: e.matmul(
                            ps[b][:, (blk % 4) * 128:(blk % 4) * 128 + 128],
                            lhsT=hT[:, kc, HALO + blk * 128:HALO + blk * 128 + 128], rhs=wv[:, kc, 2, :],
                            start=(kc == 0), stop=(kc == KC - 1)),
                            reads=[wt, ('h', kc)], writes=[('ps', b)])
                for hb in range(2):
                    OP('act', lambda e, hb=hb: e.activation(
                        out=VT[:, 4 * hb:4 * hb + 4, :], in_=ps[2 + hb][:, :].rearrange("p (b m) -> p b m", m=128),
                        func=AF.Copy),
                        reads=[('ps', 2 + hb)], writes=[('VT', m3)])
                cur.append(None)

            def stageE(hd):
                par = hd % 2
                P = hgp[par]
                ql, qb, khat, KH, qsb, e_ = (P[n] for n in ('ql', 'qb', 'khat', 'KH', 'qsb', 'e'))
                lbcol = lbc[:, j, 0, hd:hd + 1]
                omlcol = lbc[:, j, 1, hd:hd + 1]

                def et(half):
                    return ('e', par, half)

                def khtok(half, c_, a_):
                    return ('KH', par, half, c_, a_) if c_ < 4 else ('khat', par, half, a_)
                for half in range(2):
                    hs = H2[half]
                    OP('act', lambda e, hs=hs: e.activation(out=l1[:, hs], in_=e_[:, hs], func=AF.Ln, scale=lbcol,
                                                            bias=1.0),
                       reads=[et(half), 'lbc'], writes=[('l1', half)])
                    OP('act', lambda e, hs=hs: e.activation(out=l2[:, hs], in_=e_[:, hs], func=AF.Ln, bias=1.0,
                                                            scale=1.0),
                       reads=[et(half)], writes=[('l2', half)])
                    OP('dve', lambda e, hs=hs: e.tensor_tensor(out=l1[:, hs], in0=l1[:, hs], in1=l2[:, hs],
                                                               op=ALU.subtract),
                       reads=[('l1', half), ('l2', half)], writes=[('l1', half)])
                    OP('act', lambda e, hs=hs: e.activation(out=l2[:, hs], in_=l2[:, hs], func=AF.Exp, scale=-1.0),
                       reads=[('l2', half)], writes=[('l2', half)])
                    OP('dve', lambda e, hs=hs: e.scalar_tensor_tensor(out=kk[:, hs], in0=e_[:, hs], scalar=omlcol,
                                                                      in1=l2[:, hs], op0=ALU.mult, op1=ALU.mult),
                       reads=[et(half), ('l2', half), 'lbc'], writes=[('kk', half)])
                for half in range(2):
                    hs = H2[half]
                    OP('dve', lambda e, hs=hs: e.tensor_tensor_scan(out=bl[:, hs], data0=mask32[:, :], data1=l1[:, hs],
                                                                   initial=0.0, op0=ALU.mult, op1=ALU.add),
                       reads=[('l1', half), 'm32'], writes=[('bl', half)])
                for half in range(2):
                    hs = H2[half]
                    OP('dve', lambda e, hs=hs: e.tensor_tensor_scan(out=bb[:, hs], data0=mask128[:, :], data1=l1[:, hs],
                                                                   initial=0.0, op0=ALU.mult, op1=ALU.add),
                       reads=[('l1', half), 'm128'], writes=[('bb', half)])
                for half in range(2):
                    hs = H2[half]
                    bl3 = bl[:, hs].rearrange("p (c x) -> p c x", x=32)
                    e3 = e_[:, hs].rearrange("p (c x) -> p c x", x=32)
                    OP('dve', lambda e, bl3=bl3, e3=e3: e.tensor_tensor(
                        out=e3, in0=bl3[:, :, 31:32].to_broadcast([128, 16, 32]), in1=bl3, op=ALU.subtract),
                       reads=[('bl', half), et(half)], writes=[et(half)])
                for half in range(2):
                    hs = H2[half]
                    OP('act', lambda e, hs=hs: e.activation(out=e_[:, hs], in_=e_[:, hs], func=AF.Exp),
                       reads=[et(half)], writes=[et(half)])
                for half in range(2):
                    hs = H2[half]
                    OP('dve', lambda e, hs=hs: e.tensor_tensor(out=l1[:, hs], in0=kk[:, hs], in1=e_[:, hs], op=ALU.mult),
                       reads=[et(half), ('kk', half), ('l1', half)], writes=[('l1', half)])
                for half in range(2):
                    hs = H2[half]
                    bsl = slice(4 * half, 4 * half + 4)
                    Lv = bl[:, hs].rearrange("p (b a x) -> p b a x", a=4, x=32)[:, :, :, 31]
                    gt = ('Gs', half)
                    OP('dve', lambda e, Lv=Lv, bsl=bsl: e.tensor_copy(out=Gs[:, bsl, 0:3], in_=Lv[:, :, 1:4]),
                       reads=[('bl', half)], writes=[gt])
                    OP('dve', lambda e, Lv=Lv, bsl=bsl: e.tensor_tensor(out=Gs[:, bsl, 3:5], in0=Lv[:, :, 1:3],
                                                                        in1=Lv[:, :, 2:4], op=ALU.add),
                       reads=[('bl', half), gt], writes=[gt])
                    OP('dve', lambda e, Lv=Lv, bsl=bsl: e.tensor_tensor(out=Gs[:, bsl, 5:6], in0=Gs[:, bsl, 3:4],
                                                                        in1=Lv[:, :, 3:4], op=ALU.add),
                       reads=[('bl', half), gt], writes=[gt])
                    OP('act', lambda e, bsl=bsl: e.activation(out=Gs[:, bsl, :], in_=Gs[:, bsl, :], func=AF.Exp),
                       reads=[gt], writes=[gt])
                khv = khat[:, :].rearrange("p (b a x) -> p b a x", a=4, x=32)

                def kdst(a_, c_, bsl):
                    if c_ < 4:
                        return KH[:, bsl, 128 * c_ + 32 * a_:128 * c_ + 32 * a_ + 32]
                    return khv[:, bsl, a_, :]
                for (a_, c_) in ((0, 1), (1, 2), (2, 3), (3, 4)):
                    for half in range(2):
                        bsl = slice(4 * half, 4 * half + 4)
                        K1v = l1[:, H2[half]].rearrange("p (b a x) -> p b a x", a=4, x=32)
                        dst = kdst(a_, c_, bsl)
                        OP('act', lambda e, dst=dst, a_=a_, K1v=K1v: e.activation(out=dst, in_=K1v[:, :, a_, :], func=AF.Copy),
                           reads=[('l1', half)], writes=[khtok(half, c_, a_)])
                for (a_, c_, g_) in ((0, 2, 0), (1, 3, 1), (2, 4, 2), (0, 3, 3), (1, 4, 4), (0, 4, 5)):
                    for half in range(2):
                        bsl = slice(4 * half, 4 * half + 4)
                        K1v = l1[:, H2[half]].rearrange("p (b a x) -> p b a x", a=4, x=32)
                        dst = kdst(a_, c_, bsl)
                        OP('dve', lambda e, dst=dst, a_=a_, g_=g_, K1v=K1v, bsl=bsl: e.tensor_tensor(
                            out=dst, in0=K1v[:, :, a_, :], in1=Gs[:, bsl, g_:g_ + 1].to_broadcast([128, 4, 32]), op=ALU.mult),
                           reads=[('l1', half), ('Gs', half)], writes=[khtok(half, c_, a_)])
                for half in range(2):
                    hs = H2[half]
                    OP('act', lambda e, hs=hs: e.activation(out=ex[:, hs], in_=bl[:, hs], func=AF.Exp),
                       reads=[('bl', half)], writes=[('l2', half)])
                for half in range(2):
                    hs = H2[half]
                    OP('dve', lambda e, half=half, hs=hs: e.tensor_tensor(out=ql[:, hs], in0=qsb[:, hs],
                                                                          in1=ex[:, hs], op=ALU.mult),
                       reads=[('qsb', par, half), ('l2', half)], writes=[('ql', par, half)])
                for half in range(2):
                    hs = H2[half]
                    OP('act', lambda e, hs=hs: e.activation(out=e_[:, hs], in_=bl[:, hs], func=AF.Exp, scale=-1.0),
                       reads=[('bl', half), et(half)], writes=[et(half)])
                for c in range(4):
                    for half in range(2):
                        hs = H2[half]
                        bsl = slice(4 * half, 4 * half + 4)
                        OP('dve', lambda e, c=c, hs=hs, bsl=bsl: e.tensor_tensor(
                            out=KH[:, bsl, 160 * c:160 * c + 32],
                            in0=kk[:, hs].rearrange("p (b x) -> p b x", x=128)[:, :, 32 * c:32 * c + 32],
                            in1=e_[:, hs].rearrange("p (b x) -> p b x", x=128)[:, :, 32 * c:32 * c + 32], op=ALU.mult),
                           reads=[('kk', half), et(half)], writes=[khtok(half, c, c)])
                for half in range(2):
                    hs = H2[half]
                    OP('act', lambda e, hs=hs: e.activation(out=ex[:, hs], in_=bb[:, hs], func=AF.Exp),
                       reads=[('bb', half)], writes=[('l2', half)])
                for half in range(2):
                    hs = H2[half]
                    OP('dve', lambda e, half=half, hs=hs: e.tensor_tensor(out=qb[:, hs], in0=qsb[:, hs],
                                                                          in1=ex[:, hs], op=ALU.mult),
                       reads=[('qsb', par, half), ('l2', half)], writes=[('qb', par, half)])
                for half in range(2):
                    hs = H2[half]
                    bsl = slice(4 * half, 4 * half + 4)
                    OP('dve', lambda e, hs=hs, bsl=bsl: e.tensor_copy(
                        out=dcy[:, par, bsl], in_=ex[:, hs].rearrange("p (b x) -> p b x", x=128)[:, :, 127]),
                       reads=[('l2', half)], writes=[('dcy', par, half)])

            def stageB(hd):
                par = hd % 2
                P = hgp[par]
                m3 = hd % 3
                ql, qb, khat, KH = (P[n] for n in ('ql', 'qb', 'khat', 'KH'))
                sog, VT = hg3[m3]['sog'], hg3[m3]['VT']

                def tk(n):
                    return (n, par)
                pt = ps[4][:, :].bitcast(BF16)
                for blk in range(NBLK):
                    OP('pe', lambda e, blk=blk: e.transpose(out=pt[:, blk * 128:(blk + 1) * 128],
                                                              in_=khat[:, blk * 128:(blk + 1) * 128], identity=ident[:, :]),
                         reads=[('khat', par, blk // 4, a_) for a_ in range(4)] + ['ident'], writes=[('ps', 4)])
                OP('dve', lambda e: e.tensor_copy(out=KT[:, :, :], in_=pt.rearrange("p (b m) -> p b m", m=128)),
                     reads=[('ps', 4)], writes=['KT'])
                for blk in range(NBLK):
                    b = 6 + blk // 4
                    for c in range(4):
                        m = 32 * (c + 1)
                        c0 = (blk % 4) * 128 + 32 * c
                        OP('pe', lambda e, blk=blk, c=c, m=m, c0=c0, b=b: e.matmul(
                            ps[b][0:m, c0:c0 + 32], lhsT=KH[:, blk, 128 * c:128 * c + m],
                            rhs=ql[:, blk * 128 + 32 * c:blk * 128 + 32 * c + 32], start=True, stop=True),
                            reads=[('KH', par, blk // 4, c, a_) for a_ in range(c + 1)] + [('ql', par, blk // 4)],
                            writes=[('ps', b)])
                for hb in range(2):
                    for c in range(4):
                        m = 32 * (c + 1)
                        OP('dve', lambda e, hb=hb, c=c, m=m: e.tensor_tensor(
                            out=AT[0:m, 4 * hb:4 * hb + 4, 32 * c:32 * c + 32],
                            in0=ps[6 + hb][0:m, :].rearrange("p (b x) -> p b x", x=128)[:, :, 32 * c:32 * c + 32],
                            in1=cmask[0:m, 32 * c:32 * c + 32].unsqueeze(1).to_broadcast([m, 4, 32]),
                            op=ALU.mult),
                            reads=[('ps', 6 + hb), 'consts'], writes=['AT'])
                for blk in range(NBLK):
                    ub = 4 + blk % 2
                    uc = (blk // 2) * 128
                    OP('pe', lambda e, blk=blk, ub=ub, uc=uc: e.matmul(
                        ps[ub][:, uc:uc + 128], lhsT=KT[:, blk, :], rhs=VT[:, blk, :],
                        start=True, stop=True),
                        reads=['KT', ('VT', m3)], writes=[('ps', ub)])
                OP('act', lambda e: e.activation(out=Sbf8[:, 0, :], in_=Sst[:, j, hd, :], func=AF.Copy),
                     reads=[('S', hd)], writes=[('Sbf8', 0)])
                for blk in range(NBLK):
                    ub = 4 + blk % 2
                    uc = (blk // 2) * 128
                    src = Sst[:, j, hd, :] if blk % 2 == 0 else S2[:, :]
                    dst = S2[:, :] if blk % 2 == 0 else Sst[:, j, hd, :]
                    stok = ('S', hd) if blk % 2 == 0 else 'S2'
                    dtok = 'S2' if blk % 2 == 0 else ('S', hd)
                    OP('dve', lambda e, blk=blk, ub=ub, uc=uc, src=src, dst=dst: e.scalar_tensor_tensor(
                        out=dst, in0=src, scalar=dcy[:, par, blk:blk + 1],
                        in1=ps[ub][:, uc:uc + 128], op0=ALU.mult, op1=ALU.add),
                        reads=[stok, ('dcy', par, blk // 4), ('ps', ub)], writes=[dtok])
                    if blk < NBLK - 1:
                        OP('act', lambda e, blk=blk, dst=dst: e.activation(out=Sbf8[:, blk + 1, :], in_=dst, func=AF.Copy),
                             reads=[dtok], writes=[('Sbf8', blk + 1)])
                for blk in range(NBLK):
                    bo = 6 + blk // 4
                    oc = (blk % 4) * 128
                    OP('pe', lambda e, blk=blk, bo=bo, oc=oc: e.matmul(
                        ps[bo][:, oc:oc + 128], lhsT=VT[:, blk, :], rhs=AT[:, blk, :], start=True, stop=False),
                        reads=[('VT', m3), 'AT'], writes=[('ps', bo)])
                    OP('pe', lambda e, blk=blk, bo=bo, oc=oc: e.matmul(
                        ps[bo][:, oc:oc + 128], lhsT=Sbf8[:, blk, :], rhs=qb[:, blk * 128:(blk + 1) * 128],
                        start=False, stop=True),
                        reads=[('Sbf8', blk), ('qb', par, blk // 4)], writes=[('ps', bo)])
                for half in range(2):
                    hs = H2[half]
                    OP('act', lambda e, half=half, hs=hs: e.activation(out=osq_b[:, hs], in_=ps[6 + half][:, :],
                                                                         func=AF.Square),
                         reads=[('ps', 6 + half)], writes=[('osq', half)])
                    OP('pe', lambda e, half=half, hs=hs: e.matmul(ps[4 + half][:, :], lhsT=ones_bf[:, :],
                                                                    rhs=osq_b[:, hs], start=True, stop=True),
                         reads=[('osq', half), 'ones'], writes=[('ps', 4 + half)])
                    OP('act', lambda e, half=half, hs=hs: e.activation(out=rs_b[:, hs], in_=ps[4 + half][:, :],
                                                                         func=AF.Ln, scale=1.0 / 128, bias=epscol[:, 0:1]),
                         reads=[('ps', 4 + half), 'eps'], writes=[('rs', half)])
                    OP('act', lambda e, hs=hs: e.activation(out=rs_b[:, hs], in_=rs_b[:, hs], func=AF.Exp, scale=-0.5),
                         reads=[('rs', half)], writes=[('rs', half)])
                    OP('dve', lambda e, half=half, hs=hs: e.tensor_tensor(out=on_b[:, hs], in0=ps[6 + half][:, :],
                                                                            in1=rs_b[:, hs], op=ALU.mult),
                         reads=[('ps', 6 + half), ('rs', half)], writes=[('on', half)])
                    OP('dve', lambda e, hs=hs: e.scalar_tensor_tensor(out=OG[:, hd, hs], in0=on_b[:, hs],
                                                                        scalar=ng[:, hd:hd + 1], in1=sog[:, hs],
                                                                        op0=ALU.mult, op1=ALU.mult),
                         reads=[('on', half), ('sog', m3), 'cols'], writes=[('og', hd)])

            def build(fn_, hd):
                del cur[:]
                fn_(hd)
                return list(cur)

            def emit(o):
                if o is None:
                    w_release()
                else:
                    S.op(o[0], o[1], reads=o[2], writes=o[3])

            def merge(lists):
                idx = [0] * len(lists)
                while True:
                    best = None
                    bt = None
                    for li, L in enumerate(lists):
                        if idx[li] >= len(L):
                            continue
                        o = L[idx[li]]
                        t = -1.0 if o is None else S.earliest(o[0], o[2], o[3])
                        if bt is None or t < bt:
                            bt, best = t, li
                    if best is None:
                        break
                    emit(lists[best][idx[best]])
                    idx[best] += 1

            merge([build(stageP, 0)])
            merge([build(stageE, 0), build(stageP, 1)])
            for hd in range(8):
                ls = [build(stageB, hd)]
                if hd + 1 < 8:
                    ls.append(build(stageE, hd + 1))
                if hd + 2 < 8:
                    ls.append(build(stageP, hd + 2))
                merge(ls)
            for dh in range(2):
                slot, wt = w_next()
                wv = slot[:, 0:4096].rearrange("p (u k m) -> p u k m", u=4, k=KC)
                for u in range(4):
                    dt = 4 * dh + u
                    bk = [next_bank(), next_bank()]
                    for kc in range(KC):
                        for half in range(2):
                            S.op('pe', lambda e, u=u, kc=kc, half=half, bk=bk: e.matmul(
                                ps[bk[half]][:, :], lhsT=wv[:, u, kc, :], rhs=OG[:, kc, half * 512:(half + 1) * 512],
                                start=(kc == 0), stop=(kc == KC - 1)),
                                reads=[wt, ('og', kc)], writes=[('ps', bk[half])])
                    for half in range(2):
                        hs = H2[half]
                        S.op('dve', lambda e, half=half, hs=hs, dt=dt, bk=bk: e.scalar_tensor_tensor(
                            out=xT[:, dt, hs], in0=ps[bk[half]][:, :], scalar=gcol[:, dt:dt + 1], in1=xT[:, dt, hs],
                            op0=ALU.mult, op1=ALU.add),
                            reads=[('ps', bk[half]), ('x', dt), 'dcol'], writes=[('x', dt)])
                w_release()

        xsrc = xT_d.rearrange("(k p) t -> p k t", p=128)
        odst = out_d.rearrange("(k p) t -> p k t", p=128)
        fg = cols[:, coff['fgain']:coff['fgain'] + 8]
        marks = []

        def mark(lbl):
            marks.append((lbl, S.cnt['pe']))
        for tile in range(ntile):
            t0 = tile * TT
            for kc in range(KC):
                S.op('sp', lambda e, t0=t0, kc=kc: e.dma_start(out=xT[:, kc, :], in_=xsrc[:, kc, t0:t0 + TT]),
                     writes=[('x', kc)], dma='xld%d' % kc)
            for i in range(depth):
                mark('t%d L%d norm1' % (tile, i))
                norm_phase(dcol[:, i, 0, :], dcol[:, i, 1, :])
                mark('t%d L%d %s' % (tile, i, kinds[i]))
                if kinds[i] == 'pool':
                    pool_phase(i, tile)
                else:
                    hgrn_phase(i, tile)
                mark('t%d L%d norm2' % (tile, i))
                norm_phase(dcol[:, i, 3, :], dcol[:, i, 4, :])
                mark('t%d L%d ffn' % (tile, i))
                ffn_phase(i, tile)
            mark('t%d final' % tile)
            norm_phase(fg, None, final=True)
            for kc in range(KC):
                S.op('sp', lambda e, t0=t0, kc=kc: e.dma_start(out=odst[:, kc, t0:t0 + TT], in_=xT[:, kc, :]),
                     reads=[('x', kc)], writes=[('out', kc)], dma='ost%d' % kc)
        S.wait_all('sp', [('out', kc) for kc in range(KC)])
        mark('end')
        build_nc.stats = (S.n_ins, S.n_wait, dict(S.cnt))
        build_nc.marks = marks
        build_nc.sim = dict(S.eng_free)
    return nc


def make_consts():
    c = np.zeros((128, NCONST), np.float32)
    c[:, 0:128] = np.eye(128, dtype=np.float32)
    jj = np.arange(128)[:, None]
    ii = np.arange(128)[None, :]
    c[:, 128:256] = (jj <= ii).astype(np.float32)
    rc = np.zeros((4, HALO), np.float32)
    for g, w in enumerate(POOL_W):
        rc[g] = 1.0 / np.minimum(np.arange(HALO) + 1, w)
    c[:, 256:320] = rc.reshape(1, 64)
    return c


def colfmt(v):
    v = np.asarray(v, np.float32)
    return np.ascontiguousarray(v.reshape(-1, 128).T)


def host_layout(inp, depth):
    coff, ncols = cols_layout(depth)
    B = inp['x'].shape[0]
    chunks = pass_chunks(depth)
    ws = np.concatenate([host_chunk(k, i, idx, inp) for (k, i, idx, n) in chunks], axis=1)
    ws = np.ascontiguousarray(ws, dtype=np.float32)
    al = []
    for i in range(depth):
        w = inp['ada_w'][i].reshape(8, 128, 12, 512)
        al.append(w.transpose(1, 2, 0, 3).reshape(128, 12 * 4096))
    astream = np.ascontiguousarray(np.concatenate(al, axis=1), dtype=np.float32)
    consts = make_consts()
    maps = []
    for b in range(B):
        cols = np.zeros((128, ncols), np.float32)
        cols[:, coff['c']:coff['c'] + 8] = colfmt(inp['c'][b])
        for i in range(depth):
            cols[:, coff['gmix'] + 8 * i: coff['gmix'] + 8 * i + 8] = colfmt(inp['norm_mix_gain'][i])
            cols[:, coff['gffn'] + 8 * i: coff['gffn'] + 8 * i + 8] = colfmt(inp['norm_ffn_gain'][i])
        for j in range((depth + 1) // 2):
            cols[:, coff['pscale'] + 8 * j: coff['pscale'] + 8 * j + 8] = colfmt(inp['pool_scale'][j])
        for j in range(depth // 2):
            cols[:, coff['hng'] + 8 * j: coff['hng'] + 8 * j + 8] = colfmt(inp['hgrn_norm_gain'][j])
        for j in range(min(2, inp['hgrn_lb_logits'].shape[0])):
            cols[:, coff['lbl'] + 8 * j: coff['lbl'] + 8 * j + 8] = colfmt(inp['hgrn_lb_logits'][j])
        cols[:, coff['fgain']:coff['fgain'] + 8] = colfmt(inp['final_gain'])
        for i in range(depth):
            cols[:, coff['adab'] + 48 * i: coff['adab'] + 48 * i + 48] = colfmt(inp['ada_b'][i])
        maps.append({
            "xT": np.ascontiguousarray(inp['x'][b].T),
            "wstream": ws, "astream": astream, "cols": cols, "consts": consts,
        })
    return maps


def run(inp, depth, T, trace=False):
    B = inp['x'].shape[0]
    nc = build_nc(T, depth)
    maps = host_layout(inp, depth)
    res = run_bass_kernel_spmd(nc, maps, core_ids=list(range(B)), trace=trace)
    out = np.stack([np.ascontiguousarray(r["outT"].T) for r in res.results], axis=0)
    return out.astype(np.float32), res


def kernel(**inputs):
    inp = {k: np.asarray(v) for k, v in inputs.items()}
    out, _ = run(inp, 4, inp['x'].shape[1])
    return out
```
